# Optimizing a Trainium2 kernel written in Bass

```python
import jax, jax.numpy as jnp
from jax import lax
import numpy as np

D_MODEL = 2048
BATCH = 2
SEQ = 4096
DEPTH = 1

HEAD_DIM = 128
ATTN_Q_HEADS = 8
ATTN_KV_HEADS = 2
ATTN_GROUP = ATTN_Q_HEADS // ATTN_KV_HEADS
WINDOW = 128
ATTN_BLOCK = 128
ROPE_THETA = 10000.0
RET_HEADS = 8
RET_QK_DIM = 128
RET_V_DIM = 256
RET_CHUNK = 128
PEER_HEADS = 8
PEER_N_KEYS = 128
PEER_N_EXPERTS = PEER_N_KEYS * PEER_N_KEYS
PEER_TOPK = 16
PEER_KEY_DIM = 256
PEER_HALF_DIM = PEER_KEY_DIM // 2
PEER_TOKEN_BLOCK = 128
NORM_EPS = 1e-6

ATTN_Q_W = ATTN_Q_HEADS * HEAD_DIM
ATTN_KV_W = ATTN_KV_HEADS * HEAD_DIM
RET_QK_W = RET_HEADS * RET_QK_DIM
RET_V_W = RET_HEADS * RET_V_DIM
IN_WIDTHS = (ATTN_Q_W, ATTN_KV_W, ATTN_KV_W, RET_QK_W, RET_QK_W, RET_V_W, RET_V_W, D_MODEL, D_MODEL)
IN_WIDTH = ATTN_Q_W + 2 * ATTN_KV_W + 2 * RET_QK_W + 2 * RET_V_W + 2 * D_MODEL

kernel_name = "hybrid_swa_retention_peer_block"


def _split_points():
    pts, acc = [], 0
    for w in IN_WIDTHS[:-1]:
        acc += w
        pts.append(acc)
    return pts


def rmsnorm(x, g):
    xf = x.astype(jnp.float32)
    y = xf * lax.rsqrt(jnp.mean(xf * xf, axis=-1, keepdims=True) + NORM_EPS)
    return (y * g.astype(jnp.float32)).astype(x.dtype)


def rope_tables(seq):
    pos = jnp.arange(seq, dtype=jnp.float32)
    inv_freq = 1.0 / (ROPE_THETA ** (jnp.arange(0, HEAD_DIM, 2, dtype=jnp.float32) / HEAD_DIM))
    ang = pos[:, None] * inv_freq[None, :]
    return jnp.cos(ang)[:, None, :], jnp.sin(ang)[:, None, :]


def apply_rope(x, cos, sin):
    cos = cos.astype(x.dtype)
    sin = sin.astype(x.dtype)
    x1, x2 = jnp.split(x, 2, axis=-1)
    return jnp.concatenate([x1 * cos - x2 * sin, x2 * cos + x1 * sin], axis=-1)


def sliding_window_attention(q, k, v, sinks, cos, sin):
    B, S, _ = q.shape
    C = ATTN_BLOCK
    N = S // C
    q = apply_rope(q.reshape(B, S, ATTN_Q_HEADS, HEAD_DIM), cos, sin)
    k = apply_rope(k.reshape(B, S, ATTN_KV_HEADS, HEAD_DIM), cos, sin)
    v = v.reshape(B, S, ATTN_KV_HEADS, HEAD_DIM)
    qb = q.reshape(B, N, C, ATTN_KV_HEADS, ATTN_GROUP, HEAD_DIM)

    def banded(t):
        tb = t.reshape(B, N, C, ATTN_KV_HEADS, HEAD_DIM)
        prev = jnp.concatenate([jnp.zeros_like(tb[:, :1]), tb[:, :-1]], axis=1)
        return jnp.concatenate([prev, tb], axis=2)

    kb, vb = banded(k), banded(v)
    scores = jnp.einsum('bnqhgd,bnkhd->bhgnqk', qb, kb).astype(jnp.float32) * (HEAD_DIM ** -0.5)
    qi = jnp.arange(C)[:, None]
    kj = jnp.arange(2 * C)[None, :]
    diff = qi + C - kj
    blk = jnp.arange(N)[:, None, None]
    allowed = (diff >= 0) & (diff < WINDOW) & ((blk > 0) | (kj >= C))
    scores = jnp.where(allowed, scores, jnp.float32(-1e30))
    sink = sinks.astype(jnp.float32).reshape(ATTN_KV_HEADS, ATTN_GROUP)[None, :, :, None, None, None]
    sink = jnp.broadcast_to(sink, scores.shape[:-1] + (1,))
    probs = jax.nn.softmax(jnp.concatenate([scores, sink], axis=-1), axis=-1)[..., :-1]
    out = jnp.einsum('bhgnqk,bnkhd->bnqhgd', probs.astype(v.dtype), vb)
    return out.reshape(B, S, ATTN_Q_W)


def retention(q, k, v, g, cos, sin):
    B, S, _ = q.shape
    C = RET_CHUNK
    N = S // C
    H = RET_HEADS
    q = apply_rope(q.reshape(B, S, H, RET_QK_DIM), cos, sin)
    k = apply_rope(k.reshape(B, S, H, RET_QK_DIM), cos, sin) * (RET_QK_DIM ** -0.5)
    v = v.reshape(B, S, H, RET_V_DIM)
    qc = q.reshape(B, N, C, H, RET_QK_DIM).transpose(0, 3, 1, 2, 4).astype(jnp.float32)
    kc = k.reshape(B, N, C, H, RET_QK_DIM).transpose(0, 3, 1, 2, 4).astype(jnp.float32)
    vc = v.reshape(B, N, C, H, RET_V_DIM).transpose(0, 3, 1, 2, 4).astype(jnp.float32)

    log_gamma = jnp.log(1.0 - 2.0 ** (-5.0 - jnp.arange(H, dtype=jnp.float32)))
    pos = jnp.arange(C, dtype=jnp.float32)
    d = pos[:, None] - pos[None, :]
    decay_mask = jnp.where(d >= 0, jnp.exp(log_gamma[:, None, None] * jnp.maximum(d, 0.0)), 0.0)
    inner = jnp.einsum('bhncd,bhnkd->bhnck', qc, kc) * decay_mask[None, :, None]
    o_intra = jnp.einsum('bhnck,bhnke->bhnce', inner, vc)

    k_decay = jnp.exp(log_gamma[:, None] * (C - 1.0 - pos)[None, :])
    q_decay = jnp.exp(log_gamma[:, None] * (pos + 1.0)[None, :])
    chunk_decay = jnp.exp(log_gamma * C)
    kv = jnp.einsum('bhnkd,bhnke->bhnde', kc * k_decay[None, :, None, :, None], vc)

    def step(state, kv_n):
        return state * chunk_decay[None, :, None, None] + kv_n, state

    init = jnp.zeros((B, H, RET_QK_DIM, RET_V_DIM), jnp.float32)
    _, prev_states = lax.scan(step, init, kv.transpose(2, 0, 1, 3, 4))
    prev_states = prev_states.transpose(1, 2, 0, 3, 4)
    o_cross = jnp.einsum('bhncd,bhnde->bhnce', qc * q_decay[None, :, None, :, None], prev_states)
    o = (o_intra + o_cross).transpose(0, 2, 3, 1, 4).reshape(B, S, H, RET_V_DIM)
    mu = jnp.mean(o, axis=-1, keepdims=True)
    var = jnp.mean(jnp.square(o - mu), axis=-1, keepdims=True)
    o = ((o - mu) * lax.rsqrt(var + NORM_EPS)).astype(g.dtype).reshape(B, S, RET_V_W)
    return o * jax.nn.silu(g)


def peer(x, w_query, sub_keys, expert_down, expert_up):
    B, S, D = x.shape
    T = B * S
    H, K = PEER_HEADS, PEER_TOPK
    xt = x.reshape(T, D)
    qry = (xt @ w_query).reshape(T, H, 2, PEER_HALF_DIM)
    s = jnp.einsum('thpd,phnd->thpn', qry, sub_keys).astype(jnp.float32)
    top_s, top_i = lax.top_k(s, K)
    cand_s = top_s[:, :, 0, :, None] + top_s[:, :, 1, None, :]
    cand_i = top_i[:, :, 0, :, None] * PEER_N_KEYS + top_i[:, :, 1, None, :]
    best_s, best_pos = lax.top_k(cand_s.reshape(T, H, K * K), K)
    expert_idx = jnp.take_along_axis(cand_i.reshape(T, H, K * K), best_pos, axis=-1)
    gates = jax.nn.softmax(best_s, axis=-1).astype(x.dtype)
    nb = T // PEER_TOKEN_BLOCK

    def block(args):
        xb, ib, gb = args
        u = expert_down[ib]
        act = jax.nn.gelu(jnp.einsum('td,thkd->thk', xb, u), approximate=False)
        vv = expert_up[ib]
        return jnp.einsum('thk,thkd->td', gb * act, vv)

    y = lax.map(block, (xt.reshape(nb, PEER_TOKEN_BLOCK, D),
                        expert_idx.reshape(nb, PEER_TOKEN_BLOCK, H, K),
                        gates.reshape(nb, PEER_TOKEN_BLOCK, H, K)))
    return y.reshape(B, S, D)


def setup_inputs(seed: int = 0) -> dict:
    key = jax.random.key(seed)
    ks = jax.random.split(key, 14)
    f32 = jnp.float32
    L, D = DEPTH, D_MODEL
    nrm = lambda k, shape, scale: jax.random.normal(k, shape, f32) * scale
    return {
        "x": jax.random.normal(ks[0], (BATCH, SEQ, D), f32),
        "attn_norm": 1.0 + nrm(ks[1], (L, D), 0.02),
        "w_in": nrm(ks[2], (L, D, IN_WIDTH), D ** -0.5),
        "attn_sinks": nrm(ks[3], (L, ATTN_Q_HEADS), 0.5),
        "w_attn_branch": nrm(ks[4], (L, ATTN_Q_W, D), ATTN_Q_W ** -0.5),
        "w_ret_branch": nrm(ks[5], (L, RET_V_W, D), RET_V_W ** -0.5),
        "w_out": nrm(ks[6], (L, D, D), D ** -0.5),
        "ffn_norm": 1.0 + nrm(ks[7], (L, D), 0.02),
        "w_peer_query": nrm(ks[8], (L, D, PEER_HEADS * PEER_KEY_DIM), D ** -0.5),
        "peer_sub_keys": nrm(ks[9], (L, 2, PEER_HEADS, PEER_N_KEYS, PEER_HALF_DIM), PEER_HALF_DIM ** -0.5),
        "peer_expert_down": nrm(ks[10], (L, PEER_N_EXPERTS, D), D ** -0.5),
        "peer_expert_up": nrm(ks[11], (L, PEER_N_EXPERTS, D), PEER_HEADS ** -0.5),
        "final_norm": 1.0 + nrm(ks[12], (D,), 0.02),
    }


def reference(x, attn_norm, w_in, attn_sinks, w_attn_branch, w_ret_branch, w_out, ffn_norm,
              w_peer_query, peer_sub_keys, peer_expert_down, peer_expert_up, final_norm):
    S = x.shape[1]
    cos, sin = rope_tables(S)
    splits = _split_points()
    h = x
    for layer in range(DEPTH):
        xn = rmsnorm(h, attn_norm[layer])
        proj = xn @ w_in[layer]
        q_a, k_a, v_a, q_r, k_r, v_r, g_r, gate_a, gate_r = jnp.split(proj, splits, axis=-1)
        y_a = sliding_window_attention(q_a, k_a, v_a, attn_sinks[layer], cos, sin) @ w_attn_branch[layer]
        y_r = retention(q_r, k_r, v_r, g_r, cos, sin) @ w_ret_branch[layer]
        merged = jax.nn.sigmoid(gate_a) * y_a + jax.nn.sigmoid(gate_r) * y_r
        h = h + merged @ w_out[layer]
        hn = rmsnorm(h, ffn_norm[layer])
        h = h + peer(hn, w_peer_query[layer], peer_sub_keys[layer],
                     peer_expert_down[layer], peer_expert_up[layer])
    return rmsnorm(h, final_norm)
```

```python
import numpy as np
import concourse.bass as bass
import concourse.mybir as mybir
from concourse.bass_utils import run_bass_kernel_spmd

F32 = mybir.dt.float32
BF16 = mybir.dt.bfloat16
ALU = mybir.AluOpType
AF = mybir.ActivationFunctionType
AX = mybir.AxisListType

D = 2048
NT = 8
NPREV = 24
TOK = 1024
EPS = 1e-6
NEG = -1e30
INW = 11776


def K(x):
    return x if isinstance(x, (str, tuple)) else x.name


class Sched:
    def __init__(self, nc):
        self.nc = nc
        self.eng = {"pe": nc.tensor, "act": nc.scalar, "dve": nc.vector, "pool": nc.gpsimd, "sp": nc.sync}
        self.semobj = {e: nc.alloc_semaphore(name=f"s_{e}") for e in self.eng}
        self.cnt = {e: 0 for e in self.eng}
        self.seen = {e: {} for e in self.eng}
        self.lw = {}
        self.rd = {}
        self.dcnt = {}
        self.npe = 0
        self.marks = []

    def _wait(self, e, deps):
        for (sname, v) in deps:
            if e == "pe" and sname == "pe":
                continue
            if self.seen[e].get(sname, 0) < v:
                self.eng[e].wait_ge(self.semobj[sname], v)
                self.seen[e][sname] = v

    def _deps(self, reads, writes):
        d = {}

        def add(h):
            if h is None:
                return
            s, v = h
            if d.get(s, 0) < v:
                d[s] = v
        for k in reads:
            add(self.lw.get(k))
        for k in writes:
            add(self.lw.get(k))
            for h in self.rd.get(k, ()):
                add(h)
        return list(d.items())

    def _commit(self, h, reads, writes):
        for k in reads:
            self.rd.setdefault(k, []).append(h)
        for k in writes:
            self.lw[k] = h
            self.rd[k] = []

    def op(self, e, fn, reads=(), writes=()):
        reads = [K(x) for x in reads]
        writes = [K(x) for x in writes]
        self._wait(e, self._deps(reads, writes))
        inst = fn(self.eng[e])
        self.cnt[e] += 1
        inst.then_inc(self.semobj[e], 1)
        h = (e, self.cnt[e])
        self._commit(h, reads, writes)
        return h

    def dma(self, q, out, in_, reads=(), writes=(), chan=None):
        reads = [K(x) for x in reads]
        writes = [K(x) for x in writes]
        if chan is None:
            chan = writes[0] if writes else reads[0]
        cname = "d_" + str(chan)
        if cname not in self.semobj:
            self.semobj[cname] = self.nc.alloc_semaphore(name=f"dma{len(self.dcnt)}")
            self.dcnt[cname] = 0
        self._wait(q, self._deps(reads, writes))
        inst = self.eng[q].dma_start(out=out, in_=in_)
        self.dcnt[cname] += 16
        inst.then_inc(self.semobj[cname], 16)
        h = (cname, self.dcnt[cname])
        self._commit(h, reads, writes)
        return h

    def cc(self, fn, reads=(), writes=(), chan="cc"):
        reads = [K(x) for x in reads]
        writes = [K(x) for x in writes]
        cname = "d_" + str(chan)
        if cname not in self.semobj:
            self.semobj[cname] = self.nc.alloc_semaphore(name=f"cc{len(self.dcnt)}")
            self.dcnt[cname] = 0
        self._wait("pool", self._deps(reads, writes))
        inst = fn(self.eng["pool"])
        self.dcnt[cname] += 1
        inst.then_inc(self.semobj[cname])
        h = (cname, self.dcnt[cname])
        self._commit(h, reads, writes)
        return h

    def barrier(self):
        self.marks.append(self.npe)
        allh = [(e, self.cnt[e]) for e in self.eng if self.cnt[e] > 0] + list(self.dcnt.items())
        for e in self.eng:
            self._wait(e, allh)


def build_nc(stage=9):
    nc = bass.Bass("TRN2", target_bir_lowering=False)

    def din(name, shape, dt=F32):
        return nc.dram_tensor(name, list(shape), dt, kind="ExternalInput").ap()

    xo = din("xo", [TOK, D])
    xp = din("xp", [128, D])
    cs = din("cs", [1152, 128])
    wdo_d = din("wdo", [128, 8, 8])
    coef_d = din("coef", [128, 4, 8])
    tabs_d = din("tabs", [128, 40])
    maskT_d = din("maskT", [128, 8, 128])
    swam_d = din("swam", [128, 2, 256])
    ident_d = din("ident", [128, 128])
    g_attn = din("g_attn", [128, D])
    g_ffn = din("g_ffn", [128, D])
    g_fin = din("g_fin", [128, D])
    w_in_t = din("w_in", [23, 128, 16, 512])
    w_g_t = din("w_g", [32, 128, 16, 128])
    w_ab_t = din("w_ab", [8, 128, 8, 256])
    w_rb_t = din("w_rb", [8, 128, 16, 256])
    w_out_t = din("w_out", [4, 128, 16, 512])
    w_pq_t = din("w_pq", [4, 128, 16, 512])
    subk = din("subk", [2, 8, 128, 128])
    Ed_t = din("Ed", [128, 128, 16, 128])
    Eu = din("Eu", [16384, D])
    out = nc.dram_tensor("out", [TOK, D], F32, kind="ExternalOutput").ap()
    URd = nc.dram_tensor("URd", [TOK, 128, 256], BF16, kind="Internal").ap()
    Gd = nc.dram_tensor("Gd", [NT, 128, 128, 128], BF16, kind="Internal").ap()
    hd = nc.dram_tensor("hd", [TOK, D], F32, kind="Internal").ap()
    qd = nc.dram_tensor("qd", [TOK, D], BF16, kind="Internal").ap()
    Ad = nc.dram_tensor("Ad", [128, 128, TOK], BF16, kind="Internal").ap()
    SGd = nc.dram_tensor("SGd", [32, 128, TOK], BF16, kind="Internal").ap()
    Lsrc = nc.dram_tensor("Lsrc", [128, 2048], F32, kind="Internal").ap()
    Ldst = nc.dram_tensor("Ldst", [4 * 128, 2048], F32, kind="Internal").ap()

    S = Sched(nc)
    ARENA = 207 * 1024
    arena = nc.alloc_sbuf_tensor("arena", [128, ARENA // 4], F32)
    base = nc.sbuf_base - ARENA
    assert base % 32 == 0 and base > 0, base
    ps = nc.alloc_psum_tensor("ps", [128, 4096], F32)
    uid = [0]

    def at(name, off, shape, dt):
        uid[0] += 1
        nbytes = int(np.prod(shape[1:])) * (4 if dt == F32 else 2)
        assert off % 32 == 0 and off + nbytes <= ARENA, (name, off, nbytes)
        return nc.alloc_sbuf_tensor_at(f"{name}_{uid[0]}", list(shape), dt, offset=base + off)

    def bank(b, nb=1):
        return ps[:, b * 512:(b + nb) * 512]

    def bankbf(b, nb=1):
        return ps[:, b * 512:(b + nb) * 512].bitcast(BF16)

    def pk(b, nb=1):
        return [("ps", i) for i in range(b, b + nb)]

    def mm(out_ap, pairs, reads, writes, flags=None):
        def fn(e):
            n = len(pairs)
            inst = None
            for i, (l, r) in enumerate(pairs):
                st, sp = (i == 0, i == n - 1) if flags is None else flags[i]
                o = out_ap[i] if isinstance(out_ap, list) else out_ap
                inst = e.matmul(o, lhsT=l, rhs=r, start=st, stop=sp)
            S.npe += n
            return inst
        return S.op("pe", fn, reads, writes)

    def transposes(srcs, psview, identb, reads, writes):
        def fn(e):
            inst = None
            for i, s in enumerate(srcs):
                inst = e.transpose(psview[:, i, :], s, identb)
            S.npe += len(srcs)
            return inst
        return S.op("pe", fn, reads, writes)

    def act(out_ap, in_ap, func, reads, writes, **kw):
        return S.op("act", lambda e: e.activation(out=out_ap, in_=in_ap, func=func, **kw), reads, writes)

    def tt(eng, out_ap, a, b, op, reads, writes):
        return S.op(eng, lambda e: e.tensor_tensor(out=out_ap, in0=a, in1=b, op=op), reads, writes)

    def ts(eng, out_ap, a, s1, s2, op0, op1, reads, writes):
        if op1 is None:
            return S.op(eng, lambda e: e.tensor_scalar(out=out_ap, in0=a, scalar1=s1, scalar2=None, op0=op0), reads, writes)
        return S.op(eng, lambda e: e.tensor_scalar(out=out_ap, in0=a, scalar1=s1, scalar2=s2, op0=op0, op1=op1), reads, writes)

    def stt(out_ap, a, scalar, b, op0, op1, reads, writes):
        return S.op("dve", lambda e: e.scalar_tensor_tensor(out=out_ap, in0=a, scalar=scalar, in1=b, op0=op0, op1=op1), reads, writes)

    def rsqrt(ap, key):
        act(ap, ap, AF.Sqrt, [key], [key])
        S.op("dve", lambda e: e.reciprocal(out=ap, in_=ap), [key], [key])

    CONST = 34816
    identf = at("identf", 0, [128, 128], F32)
    identb = at("identb", 512, [128, 128], BF16)
    gvec = at("gvec", 768, [128, D], F32)
    cs_own = at("cs_own", 8960, [128, 8, 128], F32)
    maskT = at("maskT", 13056, [128, 8, 128], F32)
    swam = at("swam", 17152, [128, 2, 256], F32)
    tabs = at("tabs", 19200, [128, 40], F32)
    small = at("small", 19360, [128, 64], F32)
    halo_k = at("halo_k", 19616, [128, 256], BF16)
    halo_v = at("halo_v", 20128, [128, 256], BF16)
    S_f32 = at("S_f32", 20640, [128, 8, 256], F32)
    S_bf = at("S_bf", 28832, [128, 8, 256], BF16)
    junk = at("junk", 32928, [128, 16], F32)
    wdo = at("wdo", 32992, [128, 8, 8], F32)
    coef = at("coef", 33248, [128, 4, 8], F32)
    P0 = CONST

    sinkb = tabs[:, 0:8]
    kdec = tabs[:, 8:16]
    qdec = tabs[:, 16:24]
    cdec = tabs[:, 24:32]

    S.dma("sp", identf[:], ident_d, writes=[identf])
    S.dma("sp", tabs[:], tabs_d, writes=[tabs])
    S.dma("sp", maskT[:], maskT_d, writes=[maskT])
    S.dma("sp", swam[:], swam_d, writes=[swam])
    S.dma("sp", cs_own[:], cs[128:1152, :].rearrange("(j p) c -> p j c", p=128), writes=[cs_own])
    S.dma("sp", gvec[:], g_attn, writes=[gvec])
    S.dma("sp", wdo[:], wdo_d, writes=[wdo])
    S.dma("sp", coef[:], coef_d, writes=[coef])
    S.op("dve", lambda e: e.tensor_copy(out=identb[:], in_=identf[:]), [identf], [identb])


    def norm_T(x_ap, xkey, xnb, sq, dstT, dkey, tb=6):
        act(xnb[:], x_ap, AF.Square, [xkey], [xnb, (sq.name, 0)], accum_out=sq[:, 0:1])
        ts("dve", sq[:, 1:2], sq[:, 0:1], 1.0 / D, EPS, ALU.mult, ALU.add, [(sq.name, 0)], [(sq.name, 1)])
        rsqrt(sq[:, 1:2], (sq.name, 1))
        stt(xnb[:], x_ap, sq[:, 1:2], gvec[:], ALU.mult, ALU.mult, [xkey, (sq.name, 1), gvec], [xnb])
        pv = bankbf(tb, 2).rearrange("p (a b) -> p a b", b=128)
        transposes([xnb[:, c * 128:(c + 1) * 128] for c in range(16)], pv, identb[:], [xnb, identb], pk(tb, 2))
        act(dstT, pv, AF.Copy, pk(tb, 2), [dkey])

    def rope(src, skeys, H, cst, ckey, t1, t2, dst, dkey):
        dk = (lambda x: (dkey + (x,)) if isinstance(dkey, tuple) else (dkey, x))
        cosb = cst[:, 0:64].unsqueeze(1).broadcast_to([128, H, 64])
        sinb = cst[:, 64:128].unsqueeze(1).broadcast_to([128, H, 64])
        x1 = src[:, :, 0:64]
        x2 = src[:, :, 64:128]
        a = t1[:, 0:H * 64].rearrange("p (h c) -> p h c", c=64)
        b = t2[:, 0:H * 64].rearrange("p (h c) -> p h c", c=64)
        tt("dve", a, x1, cosb, ALU.mult, skeys + [ckey], [t1])
        tt("dve", b, x2, sinb, ALU.mult, skeys + [ckey], [t2])
        tt("dve", dst[:, :, 0:64], a, b, ALU.subtract, [t1, t2], [dk(0)])
        tt("dve", a, x2, cosb, ALU.mult, skeys + [ckey], [t1])
        tt("dve", b, x1, sinb, ALU.mult, skeys + [ckey], [t2])
        tt("dve", dst[:, :, 64:128], a, b, ALU.add, [t1, t2], [dk(1)])

    o = P0
    Wakv = at("Wakv", o, [128, 16, 512], BF16); o += 16384
    xt0 = at("xt0", o, [128, D], F32); o += 8192
    xnb = at("xnb", o, [128, D], BF16); o += 4096
    xTh = at("xTh", o, [128, 16, 128], BF16); o += 4096
    t1 = at("t1", o, [128, 512], F32); o += 2048
    t2 = at("t2", o, [128, 512], F32); o += 2048
    cst0 = at("cst0", o, [128, 128], F32); o += 512
    sq = at("sq", o, [128, 8], F32); o += 32
    kaf = at("kaf", o, [128, 512], F32); o += 2048
    karot = at("karot", o, [128, 2, 128], F32); o += 1024
    pb = [0]

    def nextbank():
        pb[0] ^= 1
        return 4 + pb[0]

    S.dma("pool", Wakv[:], w_in_t[2], writes=[Wakv])
    S.dma("sp", xt0[:], xp, writes=[xt0])
    S.dma("sp", cst0[:], cs[0:128, :], writes=[cst0])
    norm_T(xt0[:], xt0.name, xnb, sq, xTh[:], xTh.name)
    mm(bank(4), [(xTh[:, kc, :], Wakv[:, kc, :]) for kc in range(16)], [xTh, Wakv], pk(4))
    act(kaf[:], bank(4), AF.Copy, pk(4), [kaf])
    rope(kaf[:, 0:256].rearrange("p (h c) -> p h c", c=128), [kaf.name], 2, cst0, cst0.name, t1, t2, karot, "karot")
    S.op("dve", lambda e: e.tensor_copy(out=halo_k[:], in_=karot[:].rearrange("p h c -> p (h c)")),
         [("karot", 0), ("karot", 1)], [halo_k])
    S.op("dve", lambda e: e.tensor_copy(out=halo_v[:], in_=kaf[:, 256:512]), [kaf], [halo_v])
    S.barrier()

    xT_own = at("xT_own", P0, [128, 16, TOK], BF16)
    onm = at("onm", P0 + 32768, [128, 8, D], BF16)
    RT = P0 + 65536
    rT = at("rT", RT, [128, 16, TOK], BF16)
    aT = at("aT", P0 + 98304, [128, 8, TOK], BF16)
    TMP = P0 + 114688
    wsl = [at(f"wsl{i}", TMP + i * 16384, [128, 16, 512], BF16) for i in range(2)]
    T2 = TMP + 32768
    xt2 = at("xt2", T2 + 16384, [128, D], F32)
    xnb2 = at("xnb2", T2 + 24576, [128, D], BF16)
    sq2 = junk
    xt2b = at("xt2b", P0 + 98304, [128, D], F32)
    xnb2b = at("xnb2b", P0 + 98304 + 8192, [128, D], BF16)
    sq2b = at("sq2b", P0 + 98304 + 12288, [128, 8], F32)
    xts = [xt2, xt2b]
    xnbs = [xnb2, xnb2b]
    sqs = [sq2, sq2b]

    def own_norm(j):
        S.dma("sp", xts[j % 2][:], xo[j * 128:(j + 1) * 128, :], writes=[xts[j % 2]])
        norm_T(xts[j % 2][:], xts[j % 2].name, xnbs[j % 2], sqs[j % 2], xT_own[:, :, j * 128:(j + 1) * 128], ("xTo", j),
               tb=6 if j % 2 == 0 else 2)

    wc = [0]

    def proj_stream(col0, ncols, tiles, consume, wsrc=None, pre=None):
        for g in range(ncols // 512):
            sl = wc[0] % 2
            wc[0] += 1
            S.dma("pool", wsl[sl][:], w_in_t[col0 // 512 + g], writes=[wsl[sl]])
            for j in tiles:
                if pre is not None and g == 0:
                    pre(j)
                b = nextbank()
                mm(bank(b), [(xT_own[:, kc, j * 128:(j + 1) * 128], wsl[sl][:, kc, :]) for kc in range(16)],
                   [("xTo", j), wsl[sl]], pk(b))
                consume(g, j, b)

    q_st = at("q_st", RT, [128, 8, 1024], BF16)
    k_st = at("k_st", RT + 16384, [128, 8, 1024], BF16)
    v_st = at("v_st", P0 + 32768, [128, 8, D], BF16)
    kw_all = at("kw_all", P0 + 98304, [128, 8, 1024], BF16)
    o2 = T2
    pf = at("pf", o2, [128, 4, 128], F32); o2 += 2048
    t1b = at("t1b", o2, [128, 512], F32); o2 += 2048
    t2b = at("t2b", o2, [128, 512], F32); o2 += 2048
    qrT = at("qrT", o2, [128, 8, 128], BF16); o2 += 2048
    krT = at("krT", o2, [128, 8, 128], BF16); o2 += 2048
    kcs = at("kcs", o2, [128, 8, 128], BF16); o2 += 2048
    kd = at("kd", o2, [128, 8, 128], BF16); o2 += 2048
    mI = at("mI", o2, [128, 8, 128], BF16); o2 += 2048
    assert o2 <= ARENA
    o3 = P0 + 98304
    o_sb = at("o_sb", o3, [128, 8, 256], F32); o3 += 8192
    on0 = at("on0", o3, [128, 8, 256], F32); o3 += 8192
    S.barrier()

    def mk_rope_consumer(dst_st, name):
        def consume(g, j, b):
            act(pf[:], bank(b).rearrange("p (h c) -> p h c", c=128), AF.Copy, pk(b), [pf])
            dst = dst_st[:, j, g * 512:(g + 1) * 512].rearrange("p (h c) -> p h c", c=128)
            rope(pf[:], [pf.name], 4, cs_own[:, j, :], cs_own.name, t1b, t2b, dst, (name, j, g))
        return consume

    def consume_v(g, j, b):
        act(v_st[:, j, g * 512:(g + 1) * 512], bank(b), AF.Copy, pk(b), [("v_st", j, g)])

    allt = list(range(NT))
    for j in allt:
        own_norm(j)
    proj_stream(2560, 1024, allt, mk_rope_consumer(k_st, "k_st"))
    proj_stream(3584, 2048, allt, consume_v)
    KK = lambda j: [("k_st", j, g, x) for g in range(2) for x in range(2)]
    VK = lambda j: [("v_st", j, g) for g in range(4)]
    for j in allt:
        tt("dve", kw_all[:, j, :].rearrange("p (h c) -> p h c", c=128), k_st[:, j, :].rearrange("p (h c) -> p h c", c=128),
           wdo[:, j, :].unsqueeze(2).broadcast_to([128, 8, 128]), ALU.mult, KK(j) + [wdo], [("kw", j)])
    for h in range(8):
        mm(ps[:, h * 256:(h + 1) * 256],
           [(kw_all[:, j, h * 128:(h + 1) * 128], v_st[:, j, h * 256:(h + 1) * 256]) for j in allt],
           [("kw", j) for j in allt] + [k for j in allt for k in VK(j)], pk(h // 2))
    S2 = S_f32[:].rearrange("p h c -> p (h c)")
    act(S2, ps[:, 0:2048], AF.Copy, pk(0, 4), [S_f32])
    S.dma("sp", Lsrc, S2, reads=[S_f32], writes=["Lsrc"])
    S.cc(lambda e: e.collective_compute("AllGather", ALU.bypass, replica_groups=[[0, 1, 2, 3], [4, 5, 6, 7]],
                                        ins=[Lsrc], outs=[Ldst]), reads=["Lsrc"], writes=["Ldst"], chan="ccL")
    S.dma("pool", wsl[0][:], w_in_t[3], writes=[wsl[0]])
    S.dma("pool", wsl[1][:], w_in_t[4], writes=[wsl[1]])
    qcons = mk_rope_consumer(q_st, "q_st")

    def q_proj(j):
        for g in range(2):
            b = nextbank()
            mm(bank(b), [(xT_own[:, kc, j * 128:(j + 1) * 128], wsl[g][:, kc, :]) for kc in range(16)],
               [("xTo", j), wsl[g]], pk(b))
            qcons(g, j, b)
    q_proj(0)
    q_proj(1)
    Ldst_v = Ldst.rearrange("(r p) d -> p r d", p=128)
    O2 = o_sb[:].rearrange("p h c -> p (h c)")
    for r in range(4):
        S.dma("sp", O2, Ldst_v[:, r, :], reads=["Ldst"] + [("kw", j) for j in allt], writes=[o_sb])
        cb = coef[:, r, :].unsqueeze(2).broadcast_to([128, 8, 256])
        if r == 0:
            tt("dve", S_f32[:], o_sb[:], cb, ALU.mult, [o_sb, coef], [S_f32])
        else:
            tt("dve", on0[:], o_sb[:], cb, ALU.mult, [o_sb, coef], [on0])
            tt("dve", S_f32[:], S_f32[:], on0[:], ALU.add, [S_f32, on0], [S_f32])
    act(S_bf[:], S_f32[:], AF.Copy, [S_f32], [S_bf])

    wg = [at(f"wg{i}", T2 + 16384 + i * 4096, [128, 16, 128], BF16) for i in range(2)]
    sgs = [at(f"sgs{i}", T2 + 24576 + i * 2048, [128, TOK], BF16) for i in range(2)]

    def gates():
        for gi in range(32):
            sl = gi % 2
            S.dma("pool", wg[sl][:], w_g_t[gi], writes=[wg[sl]])
            for hf in range(2):
                tk = slice(hf * 512, (hf + 1) * 512)
                mm(bank(7), [(wg[sl][:, kc, :], xT_own[:, kc, tk]) for kc in range(16)],
                   [wg[sl]] + [("xTo", jt) for jt in range(hf * 4, hf * 4 + 4)], pk(7))
                act(sgs[sl][:, tk], bank(7), AF.Sigmoid, pk(7), [(sgs[sl].name, hf)])
                yield
            S.dma("act", SGd[gi], sgs[sl][:], reads=[(sgs[sl].name, 0), (sgs[sl].name, 1)], writes=[("SGd", gi)],
                  chan=sgs[sl].name)
    gt_ = gates()

    for j in allt:
        jj = j
        if j + 2 < NT:
            q_proj(j + 2)
        for _ in range(4):
            next(gt_, None)
        qk = [("q_st", j, g, x) for g in range(2) for x in range(2)]
        kk = KK(j)
        vk = VK(j)
        pv6 = bankbf(6).rearrange("p (a b) -> p a b", b=128)
        pv7 = bankbf(7).rearrange("p (a b) -> p a b", b=128)
        transposes([q_st[:, jj, h * 128:(h + 1) * 128] for h in range(8)], pv6[:, 0:8, :], identb[:], qk + [identb], pk(6))
        act(qrT[:], pv6[:, 0:8, :], AF.Copy, pk(6), [qrT])
        act(kcs[:], k_st[:, jj, :].rearrange("p (h c) -> p h c", c=128), AF.Copy, kk, [kcs], scale=128 ** -0.5)
        tt("dve", kd[:], k_st[:, jj, :].rearrange("p (h c) -> p h c", c=128),
           kdec.unsqueeze(2).broadcast_to([128, 8, 128]), ALU.mult, kk + [tabs], [kd])
        transposes([kcs[:, h, :] for h in range(8)], pv6[:, 0:8, :], identb[:], [kcs, identb], pk(6))
        act(krT[:], pv6[:, 0:8, :], AF.Copy, pk(6), [krT])
        mm([ps[:, 2048 + h * 128:2048 + (h + 1) * 128] for h in range(8)],
           [(krT[:, h, :], qrT[:, h, :]) for h in range(8)], [krT, qrT], pk(4, 2), flags=[(True, True)] * 8)
        tt("dve", mI[:], ps[:, 2048:3072].rearrange("p (h c) -> p h c", c=128), maskT[:], ALU.mult,
           pk(4, 2) + [maskT], [mI])
        outs, pairs, flags = [], [], []
        for h in range(8):
            outs += [ps[:, h * 256:(h + 1) * 256]] * 2
            pairs += [(mI[:, h, :], v_st[:, jj, h * 256:(h + 1) * 256]), (qrT[:, h, :], S_bf[:, h, :])]
            flags += [(True, False), (False, True)]
        mm(outs, pairs, [mI, qrT, S_bf] + vk, pk(0, 4), flags=flags)
        tt("dve", o_sb[:], ps[:, 0:2048].rearrange("p (h c) -> p h c", c=256),
           qdec.unsqueeze(2).broadcast_to([128, 8, 256]), ALU.mult, pk(0, 4) + [tabs], [o_sb])
        for _ in range(4):
            next(gt_, None)
        if j < NT - 1:
            mm([ps[:, h * 256:(h + 1) * 256] for h in range(8)],
               [(kd[:, h, :], v_st[:, jj, h * 256:(h + 1) * 256]) for h in range(8)], [kd] + vk, pk(0, 4),
               flags=[(True, True)] * 8)
            for h in range(8):
                act(S_f32[:, h, :], S_f32[:, h, :], AF.Copy, [S_f32], [S_f32], scale=float((1.0 - 2.0 ** (-5.0 - h)) ** 128))
            tt("dve", S_f32[:], S_f32[:], ps[:, 0:2048].rearrange("p (h c) -> p h c", c=256), ALU.add,
               [S_f32] + pk(0, 4), [S_f32])
            act(S_bf[:], S_f32[:], AF.Copy, [S_f32], [S_bf])
        S.op("dve", lambda e: e.tensor_reduce(out=small[:, 0:8], in_=o_sb[:], axis=AX.X, op=ALU.add), [o_sb], [("small", 0)])
        ts("dve", small[:, 0:8], small[:, 0:8], 1.0 / 256, None, ALU.mult, None, [("small", 0)], [("small", 0)])
        tt("dve", on0[:], o_sb[:], small[:, 0:8].unsqueeze(2).broadcast_to([128, 8, 256]), ALU.subtract,
           [o_sb, ("small", 0)], [on0])
        tt("pool", o_sb[:], on0[:], on0[:], ALU.mult, [on0], [o_sb])
        S.op("dve", lambda e: e.tensor_reduce(out=small[:, 8:16], in_=o_sb[:], axis=AX.X, op=ALU.add), [o_sb], [("small", 1)])
        ts("dve", small[:, 8:16], small[:, 8:16], 1.0 / 256, EPS, ALU.mult, ALU.add, [("small", 1)], [("small", 1)])
        rsqrt(small[:, 8:16], ("small", 1))
        tt("dve", onm[:, j, :].rearrange("p (h c) -> p h c", c=256), on0[:],
           small[:, 8:16].unsqueeze(2).broadcast_to([128, 8, 256]), ALU.mult, [on0, ("small", 1)], [("onm", j)] + vk)
    for _ in gt_:
        pass
    S.barrier()

    if stage <= 1:
        dbg = at("dbg", TMP, [128, D], F32)
        for j in range(NT):
            S.op("dve", lambda e: e.tensor_copy(out=dbg[:], in_=onm[:, j, :]), [("onm", j)], [dbg])
            S.dma("sp", out[j * 128:(j + 1) * 128, :], dbg[:], reads=[dbg], chan="out")
        S.barrier()
        return nc

    qa_st = at("qa_st", RT, [128, 8, 1024], BF16)
    ka_st = at("ka_st", RT + 16384, [128, 9, 256], BF16)
    va_st = at("va_st", RT + 16384 + 4608, [128, 9, 256], BF16)
    kaT = at("kaT", RT + 16384 + 9216, [128, 2, 9 * 128], BF16)
    o2 = T2
    pf2 = at("pf2", o2, [128, 4, 128], F32); o2 += 2048
    t1c = at("t1c", o2, [128, 512], F32); o2 += 2048
    t2c = at("t2c", o2, [128, 512], F32); o2 += 2048
    sm = at("sm", o2, [128, 8, 256], F32); o2 += 8192
    pe_ = at("pe_", o2, [128, 8, 256], BF16); o2 += 4096
    pT = at("pT", o2, [128, 16, 128], BF16); o2 += 4096
    qaT = at("qaT", o2, [128, 8, 128], BF16); o2 += 2048
    ao = at("ao", o2, [128, 8, 128], BF16); o2 += 2048
    assert o2 <= ARENA

    def consume_qa(g, j, b):
        act(pf2[:], bank(b).rearrange("p (h c) -> p h c", c=128), AF.Copy, pk(b), [pf2])
        dst = qa_st[:, j, g * 512:(g + 1) * 512].rearrange("p (h c) -> p h c", c=128)
        rope(pf2[:], [pf2.name], 4, cs_own[:, j, :], cs_own.name, t1c, t2c, dst, ("qa_st", j, g))
    proj_stream(0, 1024, range(NT), consume_qa)

    def consume_kva(g, j, b):
        act(pf2[:], bank(b).rearrange("p (h c) -> p h c", c=128), AF.Copy, pk(b), [pf2])
        dst = ka_st[:, j + 1, :].rearrange("p (h c) -> p h c", c=128)
        rope(pf2[:, 0:2, :], [pf2.name], 2, cs_own[:, j, :], cs_own.name, t1c, t2c, dst, ("ka_st", j + 1))
        S.op("dve", lambda e: e.tensor_copy(out=va_st[:, j + 1, :], in_=pf2[:, 2:4, :].rearrange("p h c -> p (h c)")),
             [pf2], [("va_st", j + 1)])
    proj_stream(1024, 512, range(NT), consume_kva)
    S.op("dve", lambda e: e.tensor_copy(out=ka_st[:, 0, :], in_=halo_k[:]), [halo_k], [("ka_st", 0, 0), ("ka_st", 0, 1)])
    S.op("dve", lambda e: e.tensor_copy(out=va_st[:, 0, :], in_=halo_v[:]), [halo_v], [("va_st", 0)])
    pv6 = bankbf(6).rearrange("p (a b) -> p a b", b=128)
    pv67 = bankbf(6, 2).rearrange("p (a b) -> p a b", b=128)
    for s in range(9):
        transposes([ka_st[:, s, g * 128:(g + 1) * 128] for g in range(2)], pv6[:, 0:2, :], identb[:],
                   [("ka_st", s, 0), ("ka_st", s, 1), identb], pk(6))
        act(kaT[:, :, s * 128:(s + 1) * 128], pv6[:, 0:2, :], AF.Copy, pk(6), [("kaT", s)])
    def attn_tile(j):
        qk = [("qa_st", j, g, x) for g in range(2) for x in range(2)]
        transposes([qa_st[:, j, h * 128:(h + 1) * 128] for h in range(8)], pv6[:, 0:8, :], identb[:], qk + [identb], pk(6))
        act(qaT[:], pv6[:, 0:8, :], AF.Copy, pk(6), [qaT])
        mm([ps[:, h * 256:(h + 1) * 256] for h in range(8)],
           [(qaT[:, h, :], kaT[:, h // 4, j * 128:j * 128 + 256]) for h in range(8)],
           [qaT, ("kaT", j), ("kaT", j + 1)], pk(0, 4), flags=[(True, True)] * 8)
        mk = swam[:, 0 if j == 0 else 1, :].unsqueeze(1).broadcast_to([128, 8, 256])
        stt(sm[:], ps[:, 0:2048].rearrange("p (h c) -> p h c", c=256), 128 ** -0.5, mk, ALU.mult, ALU.add,
            pk(0, 4) + [swam], [sm])
        S.op("dve", lambda e: e.tensor_reduce(out=small[:, 0:8], in_=sm[:], axis=AX.X, op=ALU.max), [sm], [("small", 0)])
        tt("dve", small[:, 0:8], small[:, 0:8], sinkb, ALU.max, [("small", 0), tabs], [("small", 0)])
        ts("dve", small[:, 0:8], small[:, 0:8], -1.0, None, ALU.mult, None, [("small", 0)], [("small", 0)])
        for h in range(8):
            act(pe_[:, h, :], sm[:, h, :], AF.Exp, [sm, ("small", 0)], [("pe_", h), ("small", 2, h)],
                bias=small[:, h:h + 1], accum_out=small[:, 16 + h:17 + h])
        tt("dve", small[:, 8:16], sinkb, small[:, 0:8], ALU.add, [("small", 0), tabs], [("small", 1)])
        act(small[:, 8:16], small[:, 8:16], AF.Exp, [("small", 1)], [("small", 1)])
        tt("dve", small[:, 8:16], small[:, 8:16], small[:, 16:24], ALU.add,
           [("small", 1)] + [("small", 2, h) for h in range(8)], [("small", 1)])
        S.op("dve", lambda e: e.reciprocal(out=small[:, 8:16], in_=small[:, 8:16]), [("small", 1)], [("small", 1)])
        transposes([pe_[:, h, kk * 128:(kk + 1) * 128] for h in range(8) for kk in range(2)], pv67, identb[:],
                   [("pe_", h) for h in range(8)] + [identb], pk(6, 2))
        act(pT[:], pv67, AF.Copy, pk(6, 2), [pT])
        outs, pairs, flags = [], [], []
        for h in range(8):
            g = h // 4
            outs += [ps[:, 2048 + h * 128:2048 + (h + 1) * 128]] * 2
            pairs += [(pT[:, 2 * h, :], va_st[:, j, g * 128:(g + 1) * 128]),
                      (pT[:, 2 * h + 1, :], va_st[:, j + 1, g * 128:(g + 1) * 128])]
            flags += [(True, False), (False, True)]
        mm(outs, pairs, [pT, ("va_st", j), ("va_st", j + 1)], pk(4, 2), flags=flags)
        tt("dve", ao[:], ps[:, 2048:3072].rearrange("p (h c) -> p h c", c=128),
           small[:, 8:16].unsqueeze(2).broadcast_to([128, 8, 128]), ALU.mult, pk(4, 2) + [("small", 1)], [ao])
        transposes([ao[:, h, :] for h in range(8)], pv6[:, 0:8, :], identb[:], [ao, identb], pk(6))
        act(aT[:, :, j * 128:(j + 1) * 128], pv6[:, 0:8, :], AF.Copy, pk(6), [("aT", j)])
    sg = at("sg", T2 + 26624, [128, 512], F32)

    def consume_g(g, j, b):
        act(sg[:], bank(b), AF.Silu, pk(b), [sg])
        tt("dve", onm[:, j, g * 512:(g + 1) * 512], onm[:, j, g * 512:(g + 1) * 512], sg[:], ALU.mult,
           [("onm", j), sg], [("onm", j)])

    def g_stream():
        for g in range(4):
            sl = wc[0] % 2
            wc[0] += 1
            S.dma("pool", wsl[sl][:], w_in_t[11 + g], writes=[wsl[sl]])
            for j in range(NT):
                b = nextbank()
                mm(bank(b), [(xT_own[:, kc, j * 128:(j + 1) * 128], wsl[sl][:, kc, :]) for kc in range(16)],
                   [("xTo", j), wsl[sl]], pk(b))
                consume_g(g, j, b)
                yield
    gs_ = g_stream()
    for j in range(NT):
        attn_tile(j)
        for _ in range(4):
            next(gs_, None)
    for _ in gs_:
        pass
    S.barrier()
    for j in range(NT):
        transposes([onm[:, j, c * 128:(c + 1) * 128] for c in range(16)], pv67, identb[:], [("onm", j), identb], pk(6, 2))
        act(rT[:, :, j * 128:(j + 1) * 128], pv67, AF.Copy, pk(6, 2), [("rT", j)])
    S.barrier()

    mT = at("mT", P0 + 32768, [128, 16, TOK], BF16)
    wm = [at(f"wm{i}", TMP + i * 12288, [128, 24, 256], BF16) for i in range(2)]
    o2 = TMP + 24576
    sab = [at(f"sab{i}", o2 + i * 1024, [128, 512], BF16) for i in range(2)]; o2 += 2048
    srb = [at(f"srb{i}", o2 + i * 1024, [128, 512], BF16) for i in range(2)]; o2 += 2048
    sa = [at(f"sa{i}", o2 + i * 2048, [128, 512], F32) for i in range(2)]; o2 += 4096
    sr = [at(f"sr{i}", o2 + i * 2048, [128, 512], F32) for i in range(2)]; o2 += 4096
    assert o2 <= ARENA
    it = [0]
    for cg in range(8):
        sl = cg % 2
        S.dma("pool", wm[sl][:, 0:8, :], w_ab_t[cg], writes=[(wm[sl].name, 0)], chan=wm[sl].name + "a")
        S.dma("pool", wm[sl][:, 8:24, :], w_rb_t[cg], writes=[(wm[sl].name, 1)], chan=wm[sl].name + "b")
        for sub in range(2):
            cc = cg * 2 + sub
            cs_ = slice(sub * 128, (sub + 1) * 128)
            for hf in range(2):
                p2 = it[0] % 2
                it[0] += 1
                b0 = 2 * (it[0] % 4)
                tk = slice(hf * 512, (hf + 1) * 512)
                tkeys = list(range(hf * 4, hf * 4 + 4))
                S.dma("sp", sab[p2][:], SGd[cc][:, tk], reads=[("SGd", cc)], writes=[sab[p2]])
                S.dma("sp", srb[p2][:], SGd[16 + cc][:, tk], reads=[("SGd", 16 + cc)], writes=[srb[p2]])
                mm(bank(b0), [(wm[sl][:, kc, cs_], aT[:, kc, tk]) for kc in range(8)],
                   [(wm[sl].name, 0)] + [("aT", j) for j in tkeys], pk(b0))
                mm(bank(b0 + 1), [(wm[sl][:, 8 + kc, cs_], rT[:, kc, tk]) for kc in range(16)],
                   [(wm[sl].name, 1)] + [("rT", j) for j in tkeys], pk(b0 + 1))
                tt("dve", sa[p2][:], sab[p2][:], bank(b0), ALU.mult, [sab[p2]] + pk(b0), [sa[p2]])
                tt("dve", sr[p2][:], srb[p2][:], bank(b0 + 1), ALU.mult, [srb[p2]] + pk(b0 + 1), [sr[p2]])
                tt("pool", mT[:, cc, tk], sa[p2][:], sr[p2][:], ALU.add, [sa[p2], sr[p2]], [("mT", cc, hf)])
    S.barrier()

    hres = at("hres", RT, [128, 8, D], F32)
    wsl2 = [at(f"wsl2{i}", RT + 65536 + i * 16384, [128, 16, 512], BF16) for i in range(2)]
    S.dma("sp", hres[:], xo.rearrange("(j p) d -> p j d", p=128), writes=[("h", j) for j in range(NT)], chan="hres")
    for g in range(4):
        sl = g % 2
        S.dma("pool", wsl2[sl][:], w_out_t[g], writes=[wsl2[sl]])
        for j in range(NT):
            b = nextbank()
            mm(bank(b), [(mT[:, kc, j * 128:(j + 1) * 128], wsl2[sl][:, kc, :]) for kc in range(16)],
               [wsl2[sl]] + [("mT", kc, j // 4) for kc in range(16)], pk(b))
            tt("dve", hres[:, j, g * 512:(g + 1) * 512], hres[:, j, g * 512:(g + 1) * 512], bank(b), ALU.add,
               [("h", j)] + pk(b), [("h", j)])
    S.barrier()
    if stage <= 2:
        S.dma("sp", out.rearrange("(j p) d -> p j d", p=128), hres[:], reads=[("h", j) for j in range(NT)], chan="out")
        S.barrier()
        return nc

    hnT = at("hnT", P0, [128, 16, TOK], BF16)
    R1 = P0 + 32768
    R2 = RT + 65536
    R2SZ = ARENA - R2
    skT = at("skT", 20640, [128, 16, 128], BF16)
    q_all = at("q_all", R1, [128, 8, D], BF16)
    wsl3 = [at(f"wsl3{i}", R2 + i * 16384, [128, 16, 512], BF16) for i in range(2)]
    xnb3 = at("xnb3", R2 + 32768, [128, D], BF16)
    skn = at("skn", R2 + 36864, [128, 16, 128], BF16)
    xnb3b = at("xnb3b", R2 + 40960, [128, D], BF16)
    sq3b = at("sq3b", R2 + 45056, [128, 8], F32)
    S.dma("sp", gvec[:], g_ffn, writes=[gvec])
    S.dma("pool", skn[:], subk.rearrange("p h n d -> n (p h) d"), writes=[skn])
    pv67 = bankbf(6, 2).rearrange("p (a b) -> p a b", b=128)
    transposes([skn[:, i, :] for i in range(16)], pv67, identb[:], [skn, identb], pk(6, 2))
    act(skT[:], pv67, AF.Copy, pk(6, 2), [skT])
    for g in range(4):
        sl = g % 2
        S.dma("pool", wsl3[sl][:], w_pq_t[g], writes=[wsl3[sl]])
        if g == 0:
            for j in range(NT):
                norm_T(hres[:, j, :], ("h", j), [xnb3, xnb3b][j % 2], [sq2, sq3b][j % 2],
                       hnT[:, :, j * 128:(j + 1) * 128], ("hnT", j), tb=6 if j % 2 == 0 else 2)
        for j in range(NT):
            b = nextbank()
            mm(bank(b), [(hnT[:, kc, j * 128:(j + 1) * 128], wsl3[sl][:, kc, :]) for kc in range(16)],
               [("hnT", j), wsl3[sl]], pk(b))
            act(q_all[:, j, g * 512:(g + 1) * 512], bank(b), AF.Copy, pk(b), [("q_all", j)])
    S.barrier()
    S.dma("sp", hd.rearrange("(j p) d -> p j d", p=128), hres[:], reads=[("h", j) for j in range(NT)], writes=["hd"], chan="hd")
    S.dma("sp", qd.rearrange("(j p) d -> p j d", p=128), q_all[:], reads=[("q_all", j) for j in range(NT)], writes=["qd"], chan="qd")
    S.barrier()
    s_sb = at("s_sb", P0 + 122880, [128, 16, 128], F32)
    D0 = [at(f"D0{i}", P0 + 131072 + i * 8192, [128, 16, 128], F32) for i in range(2)]
    URh = [at(f"URh{i}", P0 + 147456 + i * 8192, [128, 16, 256], BF16) for i in range(2)]
    EdT = [at(f"EdT{i}", P0 + 98304 + i * 4096, [128, 16, 128], BF16) for i in range(4)]
    Ag = [at(f"Ag{i}", P0 + 163840 + i * 2048, [128, TOK], BF16) for i in range(4)]
    qsl = [at(f"qsl{i}", P0 + 114688 + i * 4096, [128, D], BF16) for i in range(2)]
    qTj = at("qTj", 768, [128, 16, 128], BF16)
    Eb = [at("Eb0", 8960, [128, 16, 128], BF16), at("Eb1", 13056, [128, 16, 128], BF16)]
    vtop = at("vtop", 28832, [128, 16, 16], F32)
    cand = at("cand", 29856, [128, 16, 16], F32)
    candr = at("candr", 30880, [128, 16, 16], F32)
    best = at("best", 31904, [128, 8, 16], F32)
    rst = at("rst", 32416, [128, 64], F32)
    srep = at("srep", 24736, [128, 128], F32)
    URt = [at(f"URt{i}", P0 + 65536 + i * 8192, [128, 16, 256], BF16) for i in range(4)]
    Gt_one = at("Gt_sb0", P0 + 32768, [128, 128, 128], BF16)
    Gt_bufs = [Gt_one, Gt_one]

    def routing(j):
        qs = qsl[j % 2]
        transposes([qs[:, i * 128:(i + 1) * 128] for i in range(16)], pv67, identb[:], [qs, identb], pk(6, 2))
        act(qTj[:], pv67, AF.Copy, pk(6, 2), [qTj])
        for half in range(2):
            mm([ps[:, bl * 128:(bl + 1) * 128] for bl in range(8)],
               [(qTj[:, half * 8 + bl, :], skT[:, ((half * 8 + bl) % 2) * 8 + (half * 8 + bl) // 2, :]) for bl in range(8)],
               [qTj, skT], pk(0, 2), flags=[(True, True)] * 8)
            act(s_sb[:, half * 8:(half + 1) * 8, :].rearrange("p a b -> p (a b)"), ps[:, 0:1024], AF.Copy, pk(0, 2), [s_sb])
        for blk in range(16):
            S.op("dve", lambda e, blk=blk: e.max(out=vtop[:, blk, 0:8], in_=s_sb[:, blk, :]), [s_sb], [("vtop", blk, 0)])
            S.op("dve", lambda e, blk=blk: e.match_replace(out=srep[:], in_to_replace=vtop[:, blk, 0:8],
                                                           in_values=s_sb[:, blk, :], imm_value=NEG),
                 [s_sb, ("vtop", blk, 0)], [srep])
            S.op("dve", lambda e, blk=blk: e.max(out=vtop[:, blk, 8:16], in_=srep[:]), [srep], [("vtop", blk, 1)])
            if blk % 4 == 3:
                yield
        for h in range(8):
            vk0 = [("vtop", 2 * h, 0), ("vtop", 2 * h, 1)]
            vk1 = [("vtop", 2 * h + 1, 0), ("vtop", 2 * h + 1, 1)]
            tt("dve", cand[:], vtop[:, 2 * h, :].unsqueeze(2).broadcast_to([128, 16, 16]),
               vtop[:, 2 * h + 1, :].unsqueeze(1).broadcast_to([128, 16, 16]), ALU.add, vk0 + vk1, [cand])
            S.op("dve", lambda e, h=h: e.max(out=best[:, h, 0:8], in_=cand[:]), [cand], [("best", h, 0)])
            S.op("dve", lambda e, h=h: e.match_replace(out=candr[:], in_to_replace=best[:, h, 0:8], in_values=cand[:],
                                                       imm_value=NEG), [cand, ("best", h, 0)], [candr])
            S.op("dve", lambda e, h=h: e.max(out=best[:, h, 8:16], in_=candr[:]), [candr], [("best", h, 1)])
            if h % 2 == 1:
                yield
        bk = [("best", h, x) for h in range(8) for x in range(2)]
        ts("dve", rst[:, 0:8], best[:, :, 0], -1.0, None, ALU.mult, None, bk, [("rst", 0)])
        S.op("dve", lambda e: e.tensor_copy(out=rst[:, 24:32], in_=best[:, :, 15]), bk, [("rst", 3)])
        for h in range(8):
            act(candr[:, h, :], best[:, h, :], AF.Exp, bk + [("rst", 0)], [candr, ("rst", 1, h)],
                bias=rst[:, h:h + 1], accum_out=rst[:, 8 + h:9 + h])
        act(rst[:, 32:40], rst[:, 8:16], AF.Ln, [("rst", 1, h) for h in range(8)], [("rst", 4)])
        tt("dve", rst[:, 16:24], rst[:, 0:8], rst[:, 32:40], ALU.subtract, [("rst", 0), ("rst", 4)], [("rst", 2)])
        yield
        def dense_pre(h):
            sl = h % 2
            vk0 = [("vtop", 2 * h, 0), ("vtop", 2 * h, 1)]
            tt("dve", D0[sl][:], s_sb[:, 2 * h + 1, :].unsqueeze(1).broadcast_to([128, 16, 128]),
               vtop[:, 2 * h, :].unsqueeze(2).broadcast_to([128, 16, 128]), ALU.add, [s_sb] + vk0, [D0[sl]])
            act(Eb[sl][:], D0[sl][:], AF.Exp, [D0[sl], ("rst", 2)], [Eb[sl]], bias=rst[:, 16 + h:17 + h])
        dense_pre(0)
        for h in range(8):
            if h + 1 < 8:
                dense_pre(h + 1)
            sl = h % 2
            vk0 = [("vtop", 2 * h, 0), ("vtop", 2 * h, 1)]
            stt(URh[sl][:, :, 128:256], D0[sl][:], rst[:, 24 + h:25 + h], Eb[sl][:], ALU.is_ge, ALU.mult,
                [D0[sl], Eb[sl], ("rst", 3)], [(URh[sl].name, 1)])
            tt("dve", URh[sl][:, :, 0:128], s_sb[:, 2 * h, :].unsqueeze(1).broadcast_to([128, 16, 128]),
               vtop[:, 2 * h, :].unsqueeze(2).broadcast_to([128, 16, 128]), ALU.is_equal, [s_sb] + vk0, [(URh[sl].name, 0)])
            S.dma("pool", URd[j * 128:(j + 1) * 128, h * 16:(h + 1) * 16, :], URh[sl][:],
                  reads=[(URh[sl].name, 0), (URh[sl].name, 1)], writes=[("URd", j, h)], chan=URh[sl].name)
            yield

    gb = [0]

    def bilinear(j):
        Gt_sb = Gt_bufs[j % 2]
        gn = Gt_sb.name
        for sub in range(8):
            sl = sub % 4
            t0 = j * 128 + sub * 16
            S.dma("sp", URt[sl][:], URd[t0:t0 + 16, :, :].rearrange("t k c -> k t c"),
                  reads=[("URd", j, h) for h in range(8)], writes=[URt[sl]])
            for q4 in range(4):
                b = 4 + gb[0] % 2
                gb[0] += 1
                bv = bank(b).rearrange("p (i t) -> p i t", t=4)
                mm([bv[:, :, tq] for tq in range(4)],
                   [(URt[sl][:, q4 * 4 + tq, 128:256], URt[sl][:, q4 * 4 + tq, 0:128]) for tq in range(4)],
                   [URt[sl]], pk(b), flags=[(True, True)] * 4)
                tl = sub * 16 + q4 * 4
                act(Gt_sb[:, :, tl:tl + 4], bv, AF.Copy, pk(b), [(gn, sub, q4)])
                if q4 % 2 == 1:
                    yield
        for qq in range(4):
            S.dma("act", Gd[j, :, qq * 32:(qq + 1) * 32, :], Gt_sb[:, qq * 32:(qq + 1) * 32, :],
                  reads=[(gn, a_, b_) for a_ in range(8) for b_ in range(4)],
                  writes=[("Gd", j, qq)], chan=f"Gd{qq}_{j % 2}")
        yield

    ak = [0]

    def load_Ed(c):
        S.dma("pool", EdT[c % 4][:], Ed_t[c], writes=[EdT[c % 4]])

    def apart():
        load_Ed(0)
        load_Ed(1)
        load_Ed(2)
        for c in range(128):
            s2 = c % 4
            if c + 3 < 128:
                load_Ed(c + 3)
            for hf in range(2):
                b = 2 + ak[0] % 2
                ak[0] += 1
                tk = slice(hf * 512, (hf + 1) * 512)
                mm(bank(b), [(EdT[s2][:, kc, :], hnT[:, kc, tk]) for kc in range(16)],
                   [EdT[s2]] + [("hnT", jt) for jt in range(hf * 4, hf * 4 + 4)], pk(b))
                act(Ag[s2][:, tk], bank(b), AF.Gelu, pk(b), [(Ag[s2].name, hf)])
            S.dma("act", Ad[c, :, :], Ag[s2][:], reads=[(Ag[s2].name, 0), (Ag[s2].name, 1)], writes=[("Ad", c)], chan=Ag[s2].name)
            yield

    def load_q(j):
        S.dma("sp", qsl[j % 2][:], qd[j * 128:(j + 1) * 128, :], reads=["qd"], writes=[qsl[j % 2]])

    ap_ = apart()
    gstep = [0]
    nchunk = [0]
    load_q(0)
    for j in range(NT + 1):
        if j + 1 < NT:
            load_q(j + 1)
        r = routing(j) if j < NT else None
        b_ = bilinear(j - 1) if j >= 1 else None
        step = 0
        while r is not None or b_ is not None:
            if r is not None:
                try:
                    next(r)
                except StopIteration:
                    r = None
            if b_ is not None:
                try:
                    next(b_)
                except StopIteration:
                    b_ = None
            step += 1
            gstep[0] += 1
            while ap_ is not None and nchunk[0] < (gstep[0] * 128) // 150:
                try:
                    next(ap_)
                    nchunk[0] += 1
                except StopIteration:
                    ap_ = None
    while ap_ is not None:
        try:
            next(ap_)
        except StopIteration:
            ap_ = None
    S.barrier()
    S.dma("sp", hres[:], hd.rearrange("(j p) d -> p j d", p=128), reads=["hd"], writes=[("h", j) for j in range(NT)], chan="hres")
    S.dma("sp", gvec[:], g_fin, writes=[gvec])
    Eub = [at(f"Eub{i}", R1 + i * 16384, [128, 4, D], BF16) for i in range(2)]
    o2 = R2
    PT = [at(f"PT{i}", o2 + i * 8192, [128, 4, TOK], BF16) for i in range(2)]; o2 += 16384
    Gtc = [at(f"Gtc{i}", o2 + i * 2048, [128, TOK], BF16) for i in range(4)]; o2 += 8192
    Adc = [at(f"Adc{i}", o2 + i * 2048, [128, TOK], BF16) for i in range(4)]; o2 += 8192
    ot1 = at("ot1", o2, [128, D], F32); o2 += 8192
    assert o2 <= ARENA, o2
    Eu_v = Eu.rearrange("(c e) d -> e c d", e=128)
    yb = [0]
    NCH = 128

    def emit_P(c):
        sg_ = (c // 4) % 2
        cq = c % 4
        tt("dve", PT[sg_][:, cq, :], Adc[c % 4][:], Gtc[c % 4][:], ALU.mult, [Adc[c % 4], Gtc[c % 4]],
           [(PT[sg_].name, cq, 0), (PT[sg_].name, cq, 1)])

    def final_norm(j):
        act(ot1[:], hres[:, j, :], AF.Square, [("h", j)], [ot1, (junk.name, 0)], accum_out=sq2[:, 0:1])
        ts("dve", sq2[:, 1:2], sq2[:, 0:1], 1.0 / D, EPS, ALU.mult, ALU.add, [(junk.name, 0)], [(junk.name, 1)])
        rsqrt(sq2[:, 1:2], (junk.name, 1))
        stt(ot1[:], hres[:, j, :], sq2[:, 1:2], gvec[:], ALU.mult, ALU.mult, [("h", j), (junk.name, 1), gvec], [ot1])
        S.dma("sp", out[j * 128:(j + 1) * 128, :], ot1[:], reads=[ot1], chan="ot1o")

    def emit_B(g, last=False):
        sg_ = g % 2
        for j in range(NT):
            for db in range(4):
                b = yb[0] % 8
                yb[0] += 1
                mm(bank(b), [(PT[sg_][:, cq, j * 128:(j + 1) * 128], Eub[sg_][:, cq, db * 512:(db + 1) * 512]) for cq in range(4)],
                   [Eub[sg_]] + [(PT[sg_].name, cq, j // 4) for cq in range(4)], pk(b))
                tt("dve", hres[:, j, db * 512:(db + 1) * 512], hres[:, j, db * 512:(db + 1) * 512], bank(b), ALU.add,
                   [("h", j)] + pk(b), [("h", j)])
            if last:
                final_norm(j)

    def load_GA(c):
        S.dma("sp", Gtc[c % 4][:].rearrange("p (a b) -> p a b", b=128), Gd.rearrange("jt j i t -> j jt i t")[:, :, c, :],
              reads=[("Gd", jt, c // 32) for jt in range(NT)], writes=[Gtc[c % 4]])
        S.dma("sp", Adc[c % 4][:], Ad[c, :, :], reads=[("Ad", c)], writes=[Adc[c % 4]])

    def load_Eu(g):
        S.dma("pool", Eub[g % 2][:], Eu_v[:, g * 4:(g + 1) * 4, :], writes=[Eub[g % 2]])

    load_Eu(0)
    load_Eu(1)
    for c in range(3):
        load_GA(c)
    for c in range(NCH):
        g = c // 4
        if c + 3 < NCH:
            load_GA(c + 3)
        emit_P(c)
        if c % 4 == 3:
            emit_B(g, last=(g == NCH // 4 - 1))
            if g + 2 < NCH // 4:
                load_Eu(g + 2)
    S.barrier()
    nc._marks = S.marks
    return nc


def host_tables(p):
    f32 = np.float32
    T0 = p * 1024
    pos = np.clip(np.arange(1152) + T0 - 128, 0, None).astype(f32)
    inv_freq = (1.0 / (f32(10000.0) ** (np.arange(0, 128, 2, dtype=f32) / f32(128)))).astype(f32)
    ang = (pos[:, None] * inv_freq[None, :]).astype(f32)
    cs = np.concatenate([np.cos(ang), np.sin(ang)], axis=1).astype(f32)
    gam = 1.0 - 2.0 ** (-5.0 - np.arange(8, dtype=np.float64))
    lg = np.log(gam)
    tl = np.arange(128, dtype=np.float64)[:, None, None]
    jj = np.arange(8, dtype=np.float64)[None, :, None]
    wdo = (np.exp(lg[None, None, :] * (1023 - (128 * jj + tl))) * 128 ** -0.5).astype(f32)
    coef = np.zeros((128, 4, 8), f32)
    for r in range(4):
        if r < p:
            coef[:, r, :] = np.exp(lg * 1024.0 * (p - 1 - r))[None, :]
    posc = np.arange(128, dtype=np.float64)
    tabs = np.zeros((128, 40), f32)
    tabs[:, 8:16] = np.exp(lg[None, :] * (127 - posc)[:, None]) * 128 ** -0.5
    tabs[:, 16:24] = np.exp(lg[None, :] * (posc + 1)[:, None])
    tabs[:, 24:32] = np.exp(lg * 128)[None, :]
    k = posc[:, None, None]
    t = posc[None, None, :]
    maskT = np.where(t >= k, np.exp(-lg[None, :, None] * (k + 1)), 0.0) * np.ones((128, 8, 128))
    qi = np.arange(128)[:, None]
    kj = np.arange(256)[None, :]
    diff = qi + 128 - kj
    allowed = (diff >= 0) & (diff < 128)
    mg = np.where(allowed, 0.0, NEG).astype(f32)
    m0 = np.where(allowed & (kj >= 128), 0.0, NEG).astype(f32) if p == 0 else mg
    swam = np.stack([m0, mg], axis=1).astype(f32)
    return cs, wdo, coef, tabs, maskT.astype(f32), swam


def make_in_maps(inputs):
    f32 = np.float32
    x = np.asarray(inputs["x"], f32)
    sinks = np.asarray(inputs["attn_sinks"], f32).reshape(8)
    rep = lambda v: np.ascontiguousarray(np.broadcast_to(np.asarray(v, f32).reshape(1, D), (128, D)))
    def tile_w(w, ncol):
        w = np.asarray(w, f32)
        Kd, N = w.shape
        return np.ascontiguousarray(w.reshape(Kd // 128, 128, N // ncol, ncol).transpose(2, 1, 0, 3))

    w_in_full = np.asarray(inputs["w_in"], f32)[0]
    shared = {
        "ident": np.eye(128, dtype=f32),
        "g_attn": rep(inputs["attn_norm"]), "g_ffn": rep(inputs["ffn_norm"]), "g_fin": rep(inputs["final_norm"]),
        "w_in": tile_w(w_in_full, 512),
        "w_g": tile_w(w_in_full[:, 7680:11776], 128),
        "w_ab": tile_w(np.asarray(inputs["w_attn_branch"], f32)[0], 256),
        "w_rb": tile_w(np.asarray(inputs["w_ret_branch"], f32)[0], 256),
        "w_out": tile_w(np.asarray(inputs["w_out"], f32)[0], 512),
        "w_pq": tile_w(np.asarray(inputs["w_peer_query"], f32)[0], 512),
        "subk": np.ascontiguousarray(np.asarray(inputs["peer_sub_keys"], f32)[0]),
        "Ed": np.ascontiguousarray(np.asarray(inputs["peer_expert_down"], f32)[0].reshape(128, 128, 16, 128).transpose(0, 3, 2, 1)),
        "Eu": np.ascontiguousarray(np.asarray(inputs["peer_expert_up"], f32)[0]),
    }
    maps = []
    for c in range(8):
        b, p = c // 4, c % 4
        cs, wdo, coef, tabs, maskT, swam = host_tables(p)
        tabs[:, 0:8] = sinks[None, :]
        xprev = np.zeros((128, D), f32)
        if p:
            xprev[:] = x[b, p * 1024 - 128:p * 1024]
        m = dict(shared)
        m.update({"xo": np.ascontiguousarray(x[b, p * 1024:(p + 1) * 1024]), "xp": xprev, "cs": cs, "wdo": wdo,
                  "coef": coef, "tabs": tabs, "maskT": maskT, "swam": swam})
        maps.append(m)
    return maps


def kernel(**inputs):
    nc = build_nc()
    maps = make_in_maps(inputs)
    res = run_bass_kernel_spmd(nc, maps, core_ids=list(range(8)))
    outs = [np.asarray(r["out"], np.float32) for r in res.results]
    y = np.stack(outs, 0).reshape(2, 4, 1024, D).reshape(2, 4096, D)
    return y
```

```python
import numpy as np
import concourse.bass as bass
import concourse.mybir as mybir
from concourse.bass_utils import run_bass_kernel_spmd

F32 = mybir.dt.float32
BF16 = mybir.dt.bfloat16
ALU = mybir.AluOpType
AF = mybir.ActivationFunctionType
AX = mybir.AxisListType

D = 2048
NT = 8
NPREV = 24
TOK = 1024
EPS = 1e-6
NEG = -1e30
INW = 11776


def K(x):
    return x if isinstance(x, (str, tuple)) else x.name


class Sched:
    def __init__(self, nc):
        self.nc = nc
        self.eng = {"pe": nc.tensor, "act": nc.scalar, "dve": nc.vector, "pool": nc.gpsimd, "sp": nc.sync}
        self.semobj = {e: nc.alloc_semaphore(name=f"s_{e}") for e in self.eng}
        self.cnt = {e: 0 for e in self.eng}
        self.seen = {e: {} for e in self.eng}
        self.lw = {}
        self.rd = {}
        self.dcnt = {}
        self.npe = 0
        self.marks = []

    def _wait(self, e, deps):
        for (sname, v) in deps:
            if e == "pe" and sname == "pe":
                continue
            if self.seen[e].get(sname, 0) < v:
                self.eng[e].wait_ge(self.semobj[sname], v)
                self.seen[e][sname] = v

    def _deps(self, reads, writes):
        d = {}

        def add(h):
            if h is None:
                return
            s, v = h
            if d.get(s, 0) < v:
                d[s] = v
        for k in reads:
            add(self.lw.get(k))
        for k in writes:
            add(self.lw.get(k))
            for h in self.rd.get(k, ()):
                add(h)
        return list(d.items())

    def _commit(self, h, reads, writes):
        for k in reads:
            self.rd.setdefault(k, []).append(h)
        for k in writes:
            self.lw[k] = h
            self.rd[k] = []

    def op(self, e, fn, reads=(), writes=()):
        reads = [K(x) for x in reads]
        writes = [K(x) for x in writes]
        self._wait(e, self._deps(reads, writes))
        inst = fn(self.eng[e])
        self.cnt[e] += 1
        inst.then_inc(self.semobj[e], 1)
        h = (e, self.cnt[e])
        self._commit(h, reads, writes)
        return h

    def dma(self, q, out, in_, reads=(), writes=(), chan=None):
        reads = [K(x) for x in reads]
        writes = [K(x) for x in writes]
        if chan is None:
            chan = writes[0] if writes else reads[0]
        cname = "d_" + str(chan)
        if cname not in self.semobj:
            self.semobj[cname] = self.nc.alloc_semaphore(name=f"dma{len(self.dcnt)}")
            self.dcnt[cname] = 0
        self._wait(q, self._deps(reads, writes))
        inst = self.eng[q].dma_start(out=out, in_=in_)
        self.dcnt[cname] += 16
        inst.then_inc(self.semobj[cname], 16)
        h = (cname, self.dcnt[cname])
        self._commit(h, reads, writes)
        return h

    def cc(self, fn, reads=(), writes=(), chan="cc"):
        reads = [K(x) for x in reads]
        writes = [K(x) for x in writes]
        cname = "d_" + str(chan)
        if cname not in self.semobj:
            self.semobj[cname] = self.nc.alloc_semaphore(name=f"cc{len(self.dcnt)}")
            self.dcnt[cname] = 0
        self._wait("pool", self._deps(reads, writes))
        inst = fn(self.eng["pool"])
        self.dcnt[cname] += 1
        inst.then_inc(self.semobj[cname])
        h = (cname, self.dcnt[cname])
        self._commit(h, reads, writes)
        return h

    def barrier(self):
        self.marks.append(self.npe)
        allh = [(e, self.cnt[e]) for e in self.eng if self.cnt[e] > 0] + list(self.dcnt.items())
        for e in self.eng:
            self._wait(e, allh)


def build_nc(stage=9):
    nc = bass.Bass("TRN2", target_bir_lowering=False)

    def din(name, shape, dt=F32):
        return nc.dram_tensor(name, list(shape), dt, kind="ExternalInput").ap()

    xo = din("xo", [TOK, D])
    xp = din("xp", [128, D])
    cs = din("cs", [1152, 128])
    wdo_d = din("wdo", [128, 8, 8])
    coef_d = din("coef", [128, 4, 8])
    tabs_d = din("tabs", [128, 40])
    maskT_d = din("maskT", [128, 8, 128])
    swam_d = din("swam", [128, 2, 256])
    ident_d = din("ident", [128, 128])
    g_attn = din("g_attn", [128, D])
    g_ffn = din("g_ffn", [128, D])
    g_fin = din("g_fin", [128, D])
    w_in_t = din("w_in", [23, 128, 16, 512])
    w_g_t = din("w_g", [16, 128, 16, 256])
    w_ab_t = din("w_ab", [8, 128, 8, 256])
    w_rb_t = din("w_rb", [8, 128, 16, 256])
    w_out_t = din("w_out", [4, 128, 16, 512])
    w_pq_t = din("w_pq", [4, 128, 16, 512])
    subk = din("subk", [2, 8, 128, 128])
    Ed_t = din("Ed", [128, 128, 16, 128])
    Eu = din("Eu", [16384, D])
    out = nc.dram_tensor("out", [TOK, D], F32, kind="ExternalOutput").ap()
    URd = nc.dram_tensor("URd", [TOK, 128, 256], BF16, kind="Internal").ap()
    Gd = nc.dram_tensor("Gd", [NT, 128, 128, 128], BF16, kind="Internal").ap()
    hd = nc.dram_tensor("hd", [TOK, D], F32, kind="Internal").ap()
    qd = nc.dram_tensor("qd", [TOK, D], BF16, kind="Internal").ap()
    Ad = nc.dram_tensor("Ad", [128, 128, TOK], BF16, kind="Internal").ap()
    Lsrc = nc.dram_tensor("Lsrc", [128, 2048], F32, kind="Internal").ap()
    Ldst = nc.dram_tensor("Ldst", [4 * 128, 2048], F32, kind="Internal").ap()

    S = Sched(nc)
    ARENA = 207 * 1024
    arena = nc.alloc_sbuf_tensor("arena", [128, ARENA // 4], F32)
    base = nc.sbuf_base - ARENA
    assert base % 32 == 0 and base > 0, base
    ps = nc.alloc_psum_tensor("ps", [128, 4096], F32)
    uid = [0]

    def at(name, off, shape, dt):
        uid[0] += 1
        nbytes = int(np.prod(shape[1:])) * (4 if dt == F32 else 2)
        assert off % 32 == 0 and off + nbytes <= ARENA, (name, off, nbytes)
        return nc.alloc_sbuf_tensor_at(f"{name}_{uid[0]}", list(shape), dt, offset=base + off)

    def bank(b, nb=1):
        return ps[:, b * 512:(b + nb) * 512]

    def bankbf(b, nb=1):
        return ps[:, b * 512:(b + nb) * 512].bitcast(BF16)

    def pk(b, nb=1):
        return [("ps", i) for i in range(b, b + nb)]

    def mm(out_ap, pairs, reads, writes, flags=None):
        def fn(e):
            n = len(pairs)
            inst = None
            for i, (l, r) in enumerate(pairs):
                st, sp = (i == 0, i == n - 1) if flags is None else flags[i]
                o = out_ap[i] if isinstance(out_ap, list) else out_ap
                inst = e.matmul(o, lhsT=l, rhs=r, start=st, stop=sp)
            S.npe += n
            return inst
        return S.op("pe", fn, reads, writes)

    def transposes(srcs, psview, identb, reads, writes):
        def fn(e):
            inst = None
            for i, s in enumerate(srcs):
                inst = e.transpose(psview[:, i, :], s, identb)
            S.npe += len(srcs)
            return inst
        return S.op("pe", fn, reads, writes)

    def act(out_ap, in_ap, func, reads, writes, **kw):
        return S.op("act", lambda e: e.activation(out=out_ap, in_=in_ap, func=func, **kw), reads, writes)

    def tt(eng, out_ap, a, b, op, reads, writes):
        return S.op(eng, lambda e: e.tensor_tensor(out=out_ap, in0=a, in1=b, op=op), reads, writes)

    def ts(eng, out_ap, a, s1, s2, op0, op1, reads, writes):
        if op1 is None:
            return S.op(eng, lambda e: e.tensor_scalar(out=out_ap, in0=a, scalar1=s1, scalar2=None, op0=op0), reads, writes)
        return S.op(eng, lambda e: e.tensor_scalar(out=out_ap, in0=a, scalar1=s1, scalar2=s2, op0=op0, op1=op1), reads, writes)

    def stt(out_ap, a, scalar, b, op0, op1, reads, writes):
        return S.op("dve", lambda e: e.scalar_tensor_tensor(out=out_ap, in0=a, scalar=scalar, in1=b, op0=op0, op1=op1), reads, writes)

    def rsqrt(ap, key):
        act(ap, ap, AF.Sqrt, [key], [key])
        S.op("dve", lambda e: e.reciprocal(out=ap, in_=ap), [key], [key])

    CONST = 34816
    identf = at("identf", 0, [128, 128], F32)
    identb = at("identb", 512, [128, 128], BF16)
    gvec = at("gvec", 768, [128, D], F32)
    cs_own = at("cs_own", 8960, [128, 8, 128], F32)
    maskT = at("maskT", 13056, [128, 8, 128], F32)
    swam = at("swam", 17152, [128, 2, 256], F32)
    tabs = at("tabs", 19200, [128, 40], F32)
    small = at("small", 19360, [128, 64], F32)
    halo_k = at("halo_k", 19616, [128, 256], BF16)
    halo_v = at("halo_v", 20128, [128, 256], BF16)
    S_f32 = at("S_f32", 20640, [128, 8, 256], F32)
    S_bf = at("S_bf", 28832, [128, 8, 256], BF16)
    junk = at("junk", 32928, [128, 16], F32)
    wdo = at("wdo", 32992, [128, 8, 8], F32)
    coef = at("coef", 33248, [128, 4, 8], F32)
    P0 = CONST

    sinkb = tabs[:, 0:8]
    kdec = tabs[:, 8:16]
    qdec = tabs[:, 16:24]
    cdec = tabs[:, 24:32]

    S.dma("sp", identf[:], ident_d, writes=[identf])
    S.dma("sp", tabs[:], tabs_d, writes=[tabs])
    S.dma("sp", maskT[:], maskT_d, writes=[maskT])
    S.dma("sp", swam[:], swam_d, writes=[swam])
    S.dma("sp", cs_own[:], cs[128:1152, :].rearrange("(j p) c -> p j c", p=128), writes=[cs_own])
    S.dma("sp", gvec[:], g_attn, writes=[gvec])
    S.dma("sp", wdo[:], wdo_d, writes=[wdo])
    S.dma("sp", coef[:], coef_d, writes=[coef])
    S.op("dve", lambda e: e.tensor_copy(out=identb[:], in_=identf[:]), [identf], [identb])


    def norm_T(x_ap, xkey, xnb, sq, dstT, dkey, tb=6):
        act(xnb[:], x_ap, AF.Square, [xkey], [xnb, (sq.name, 0)], accum_out=sq[:, 0:1])
        ts("dve", sq[:, 1:2], sq[:, 0:1], 1.0 / D, EPS, ALU.mult, ALU.add, [(sq.name, 0)], [(sq.name, 1)])
        rsqrt(sq[:, 1:2], (sq.name, 1))
        stt(xnb[:], x_ap, sq[:, 1:2], gvec[:], ALU.mult, ALU.mult, [xkey, (sq.name, 1), gvec], [xnb])
        pv = bankbf(tb, 2).rearrange("p (a b) -> p a b", b=128)
        transposes([xnb[:, c * 128:(c + 1) * 128] for c in range(16)], pv, identb[:], [xnb, identb], pk(tb, 2))
        act(dstT, pv, AF.Copy, pk(tb, 2), [dkey])

    def rope(src, skeys, H, cst, ckey, t1, t2, dst, dkey):
        dk = (lambda x: (dkey + (x,)) if isinstance(dkey, tuple) else (dkey, x))
        cosb = cst[:, 0:64].unsqueeze(1).broadcast_to([128, H, 64])
        sinb = cst[:, 64:128].unsqueeze(1).broadcast_to([128, H, 64])
        x1 = src[:, :, 0:64]
        x2 = src[:, :, 64:128]
        a = t1[:, 0:H * 64].rearrange("p (h c) -> p h c", c=64)
        b = t2[:, 0:H * 64].rearrange("p (h c) -> p h c", c=64)
        tt("dve", a, x1, cosb, ALU.mult, skeys + [ckey], [t1])
        tt("dve", b, x2, sinb, ALU.mult, skeys + [ckey], [t2])
        tt("dve", dst[:, :, 0:64], a, b, ALU.subtract, [t1, t2], [dk(0)])
        tt("dve", a, x2, cosb, ALU.mult, skeys + [ckey], [t1])
        tt("dve", b, x1, sinb, ALU.mult, skeys + [ckey], [t2])
        tt("dve", dst[:, :, 64:128], a, b, ALU.add, [t1, t2], [dk(1)])

    o = P0
    Wakv = at("Wakv", o, [128, 16, 512], BF16); o += 16384
    xt0 = at("xt0", o, [128, D], F32); o += 8192
    xnb = at("xnb", o, [128, D], BF16); o += 4096
    xTh = at("xTh", o, [128, 16, 128], BF16); o += 4096
    t1 = at("t1", o, [128, 512], F32); o += 2048
    t2 = at("t2", o, [128, 512], F32); o += 2048
    cst0 = at("cst0", o, [128, 128], F32); o += 512
    sq = at("sq", o, [128, 8], F32); o += 32
    kaf = at("kaf", o, [128, 512], F32); o += 2048
    karot = at("karot", o, [128, 2, 128], F32); o += 1024
    pb = [0]

    def nextbank():
        pb[0] ^= 1
        return 4 + pb[0]

    S.dma("pool", Wakv[:], w_in_t[2], writes=[Wakv])
    S.dma("sp", xt0[:], xp, writes=[xt0])
    S.dma("sp", cst0[:], cs[0:128, :], writes=[cst0])
    norm_T(xt0[:], xt0.name, xnb, sq, xTh[:], xTh.name)
    mm(bank(4), [(xTh[:, kc, :], Wakv[:, kc, :]) for kc in range(16)], [xTh, Wakv], pk(4))
    act(kaf[:], bank(4), AF.Copy, pk(4), [kaf])
    rope(kaf[:, 0:256].rearrange("p (h c) -> p h c", c=128), [kaf.name], 2, cst0, cst0.name, t1, t2, karot, "karot")
    S.op("dve", lambda e: e.tensor_copy(out=halo_k[:], in_=karot[:].rearrange("p h c -> p (h c)")),
         [("karot", 0), ("karot", 1)], [halo_k])
    S.op("dve", lambda e: e.tensor_copy(out=halo_v[:], in_=kaf[:, 256:512]), [kaf], [halo_v])
    S.barrier()

    xT_own = at("xT_own", P0, [128, 16, TOK], BF16)
    onm = at("onm", P0 + 32768, [128, 8, D], BF16)
    RT = P0 + 65536
    rT = at("rT", RT, [128, 16, TOK], BF16)
    aT = at("aT", P0 + 98304, [128, 8, TOK], BF16)
    TMP = P0 + 114688
    wsl = [at(f"wsl{i}", TMP + i * 16384, [128, 16, 512], BF16) for i in range(2)]
    T2 = TMP + 32768
    xt2 = at("xt2", T2 + 16384, [128, D], F32)
    xnb2 = at("xnb2", T2 + 24576, [128, D], BF16)
    sq2 = junk
    xt2b = at("xt2b", P0 + 98304, [128, D], F32)
    xnb2b = at("xnb2b", P0 + 98304 + 8192, [128, D], BF16)
    sq2b = at("sq2b", P0 + 98304 + 12288, [128, 8], F32)
    xts = [xt2, xt2b]
    xnbs = [xnb2, xnb2b]
    sqs = [sq2, sq2b]

    def own_norm(j):
        S.dma("sp", xts[j % 2][:], xo[j * 128:(j + 1) * 128, :], writes=[xts[j % 2]])
        norm_T(xts[j % 2][:], xts[j % 2].name, xnbs[j % 2], sqs[j % 2], xT_own[:, :, j * 128:(j + 1) * 128], ("xTo", j),
               tb=6 if j % 2 == 0 else 2)

    wc = [0]

    def proj_stream(col0, ncols, tiles, consume, wsrc=None, pre=None):
        for g in range(ncols // 512):
            sl = wc[0] % 2
            wc[0] += 1
            S.dma("pool", wsl[sl][:], w_in_t[col0 // 512 + g], writes=[wsl[sl]])
            for j in tiles:
                if pre is not None and g == 0:
                    pre(j)
                b = nextbank()
                mm(bank(b), [(xT_own[:, kc, j * 128:(j + 1) * 128], wsl[sl][:, kc, :]) for kc in range(16)],
                   [("xTo", j), wsl[sl]], pk(b))
                consume(g, j, b)

    q_st = at("q_st", RT, [128, 8, 1024], BF16)
    k_st = at("k_st", RT + 16384, [128, 8, 1024], BF16)
    v_st = at("v_st", P0 + 32768, [128, 8, D], BF16)
    kw_all = at("kw_all", P0 + 98304, [128, 8, 1024], BF16)
    o2 = T2
    pf = at("pf", o2, [128, 4, 128], F32); o2 += 2048
    t1b = at("t1b", o2, [128, 512], F32); o2 += 2048
    t2b = at("t2b", o2, [128, 512], F32); o2 += 2048
    qrT = at("qrT", o2, [128, 8, 128], BF16); o2 += 2048
    krT = at("krT", o2, [128, 8, 128], BF16); o2 += 2048
    kcs = at("kcs", o2, [128, 8, 128], BF16); o2 += 2048
    kd = at("kd", o2, [128, 8, 128], BF16); o2 += 2048
    mI = at("mI", o2, [128, 8, 128], BF16); o2 += 2048
    assert o2 <= ARENA
    o3 = P0 + 98304
    o_sb = at("o_sb", o3, [128, 8, 256], F32); o3 += 8192
    on0 = at("on0", o3, [128, 8, 256], F32); o3 += 8192
    S.barrier()

    def mk_rope_consumer(dst_st, name):
        def consume(g, j, b):
            act(pf[:], bank(b).rearrange("p (h c) -> p h c", c=128), AF.Copy, pk(b), [pf])
            dst = dst_st[:, j, g * 512:(g + 1) * 512].rearrange("p (h c) -> p h c", c=128)
            rope(pf[:], [pf.name], 4, cs_own[:, j, :], cs_own.name, t1b, t2b, dst, (name, j, g))
        return consume

    def consume_v(g, j, b):
        act(v_st[:, j, g * 512:(g + 1) * 512], bank(b), AF.Copy, pk(b), [("v_st", j, g)])

    allt = list(range(NT))
    for j in allt:
        own_norm(j)
    proj_stream(2560, 1024, allt, mk_rope_consumer(k_st, "k_st"))
    proj_stream(3584, 2048, allt, consume_v)
    KK = lambda j: [("k_st", j, g, x) for g in range(2) for x in range(2)]
    VK = lambda j: [("v_st", j, g) for g in range(4)]
    for j in allt:
        tt("dve", kw_all[:, j, :].rearrange("p (h c) -> p h c", c=128), k_st[:, j, :].rearrange("p (h c) -> p h c", c=128),
           wdo[:, j, :].unsqueeze(2).broadcast_to([128, 8, 128]), ALU.mult, KK(j) + [wdo], [("kw", j)])
    for h in range(8):
        mm(ps[:, h * 256:(h + 1) * 256],
           [(kw_all[:, j, h * 128:(h + 1) * 128], v_st[:, j, h * 256:(h + 1) * 256]) for j in allt],
           [("kw", j) for j in allt] + [k for j in allt for k in VK(j)], pk(h // 2))
    S2 = S_f32[:].rearrange("p h c -> p (h c)")
    act(S2, ps[:, 0:2048], AF.Copy, pk(0, 4), [S_f32])
    S.dma("sp", Lsrc, S2, reads=[S_f32], writes=["Lsrc"])
    S.cc(lambda e: e.collective_compute("AllGather", ALU.bypass, replica_groups=[[0, 1, 2, 3], [4, 5, 6, 7]],
                                        ins=[Lsrc], outs=[Ldst]), reads=["Lsrc"], writes=["Ldst"], chan="ccL")
    S.dma("pool", wsl[0][:], w_in_t[3], writes=[wsl[0]])
    S.dma("pool", wsl[1][:], w_in_t[4], writes=[wsl[1]])
    qcons = mk_rope_consumer(q_st, "q_st")

    def q_proj(j):
        for g in range(2):
            b = nextbank()
            mm(bank(b), [(xT_own[:, kc, j * 128:(j + 1) * 128], wsl[g][:, kc, :]) for kc in range(16)],
               [("xTo", j), wsl[g]], pk(b))
            qcons(g, j, b)
    q_proj(0)
    q_proj(1)
    Ldst_v = Ldst.rearrange("(r p) d -> p r d", p=128)
    O2 = o_sb[:].rearrange("p h c -> p (h c)")
    for r in range(4):
        S.dma("sp", O2, Ldst_v[:, r, :], reads=["Ldst"] + [("kw", j) for j in allt], writes=[o_sb])
        cb = coef[:, r, :].unsqueeze(2).broadcast_to([128, 8, 256])
        if r == 0:
            tt("dve", S_f32[:], o_sb[:], cb, ALU.mult, [o_sb, coef], [S_f32])
        else:
            tt("dve", on0[:], o_sb[:], cb, ALU.mult, [o_sb, coef], [on0])
            tt("dve", S_f32[:], S_f32[:], on0[:], ALU.add, [S_f32, on0], [S_f32])
    act(S_bf[:], S_f32[:], AF.Copy, [S_f32], [S_bf])

    for j in allt:
        jj = j
        if j + 2 < NT:
            q_proj(j + 2)
        qk = [("q_st", j, g, x) for g in range(2) for x in range(2)]
        kk = KK(j)
        vk = VK(j)
        pv6 = bankbf(6).rearrange("p (a b) -> p a b", b=128)
        pv7 = bankbf(7).rearrange("p (a b) -> p a b", b=128)
        transposes([q_st[:, jj, h * 128:(h + 1) * 128] for h in range(8)], pv6[:, 0:8, :], identb[:], qk + [identb], pk(6))
        act(qrT[:], pv6[:, 0:8, :], AF.Copy, pk(6), [qrT])
        act(kcs[:], k_st[:, jj, :].rearrange("p (h c) -> p h c", c=128), AF.Copy, kk, [kcs], scale=128 ** -0.5)
        tt("dve", kd[:], k_st[:, jj, :].rearrange("p (h c) -> p h c", c=128),
           kdec.unsqueeze(2).broadcast_to([128, 8, 128]), ALU.mult, kk + [tabs], [kd])
        transposes([kcs[:, h, :] for h in range(8)], pv7[:, 0:8, :], identb[:], [kcs, identb], pk(7))
        act(krT[:], pv7[:, 0:8, :], AF.Copy, pk(7), [krT])
        mm([ps[:, 2048 + h * 128:2048 + (h + 1) * 128] for h in range(8)],
           [(krT[:, h, :], qrT[:, h, :]) for h in range(8)], [krT, qrT], pk(4, 2), flags=[(True, True)] * 8)
        tt("dve", mI[:], ps[:, 2048:3072].rearrange("p (h c) -> p h c", c=128), maskT[:], ALU.mult,
           pk(4, 2) + [maskT], [mI])
        outs, pairs, flags = [], [], []
        for h in range(8):
            outs += [ps[:, h * 256:(h + 1) * 256]] * 2
            pairs += [(mI[:, h, :], v_st[:, jj, h * 256:(h + 1) * 256]), (qrT[:, h, :], S_bf[:, h, :])]
            flags += [(True, False), (False, True)]
        mm(outs, pairs, [mI, qrT, S_bf] + vk, pk(0, 4), flags=flags)
        tt("dve", o_sb[:], ps[:, 0:2048].rearrange("p (h c) -> p h c", c=256),
           qdec.unsqueeze(2).broadcast_to([128, 8, 256]), ALU.mult, pk(0, 4) + [tabs], [o_sb])
        if j < NT - 1:
            mm([ps[:, h * 256:(h + 1) * 256] for h in range(8)],
               [(kd[:, h, :], v_st[:, jj, h * 256:(h + 1) * 256]) for h in range(8)], [kd] + vk, pk(0, 4),
               flags=[(True, True)] * 8)
            for h in range(8):
                act(S_f32[:, h, :], S_f32[:, h, :], AF.Copy, [S_f32], [S_f32], scale=float((1.0 - 2.0 ** (-5.0 - h)) ** 128))
            tt("dve", S_f32[:], S_f32[:], ps[:, 0:2048].rearrange("p (h c) -> p h c", c=256), ALU.add,
               [S_f32] + pk(0, 4), [S_f32])
            act(S_bf[:], S_f32[:], AF.Copy, [S_f32], [S_bf])
        S.op("dve", lambda e: e.tensor_reduce(out=small[:, 0:8], in_=o_sb[:], axis=AX.X, op=ALU.add), [o_sb], [("small", 0)])
        ts("dve", small[:, 0:8], small[:, 0:8], 1.0 / 256, None, ALU.mult, None, [("small", 0)], [("small", 0)])
        tt("dve", on0[:], o_sb[:], small[:, 0:8].unsqueeze(2).broadcast_to([128, 8, 256]), ALU.subtract,
           [o_sb, ("small", 0)], [on0])
        tt("pool", o_sb[:], on0[:], on0[:], ALU.mult, [on0], [o_sb])
        S.op("dve", lambda e: e.tensor_reduce(out=small[:, 8:16], in_=o_sb[:], axis=AX.X, op=ALU.add), [o_sb], [("small", 1)])
        ts("dve", small[:, 8:16], small[:, 8:16], 1.0 / 256, EPS, ALU.mult, ALU.add, [("small", 1)], [("small", 1)])
        rsqrt(small[:, 8:16], ("small", 1))
        tt("dve", onm[:, j, :].rearrange("p (h c) -> p h c", c=256), on0[:],
           small[:, 8:16].unsqueeze(2).broadcast_to([128, 8, 256]), ALU.mult, [on0, ("small", 1)], [("onm", j)] + vk)
    S.barrier()

    if stage <= 1:
        dbg = at("dbg", TMP, [128, D], F32)
        for j in range(NT):
            S.op("dve", lambda e: e.tensor_copy(out=dbg[:], in_=onm[:, j, :]), [("onm", j)], [dbg])
            S.dma("sp", out[j * 128:(j + 1) * 128, :], dbg[:], reads=[dbg], chan="out")
        S.barrier()
        return nc

    qa_st = at("qa_st", RT, [128, 8, 1024], BF16)
    ka_st = at("ka_st", RT + 16384, [128, 9, 256], BF16)
    va_st = at("va_st", RT + 16384 + 4608, [128, 9, 256], BF16)
    kaT = at("kaT", RT + 16384 + 9216, [128, 2, 9 * 128], BF16)
    o2 = T2
    pf2 = at("pf2", o2, [128, 4, 128], F32); o2 += 2048
    t1c = at("t1c", o2, [128, 512], F32); o2 += 2048
    t2c = at("t2c", o2, [128, 512], F32); o2 += 2048
    sm = at("sm", o2, [128, 8, 256], F32); o2 += 8192
    pe_ = at("pe_", o2, [128, 8, 256], BF16); o2 += 4096
    pT = at("pT", o2, [128, 16, 128], BF16); o2 += 4096
    qaT = at("qaT", o2, [128, 8, 128], BF16); o2 += 2048
    ao = at("ao", o2, [128, 8, 128], BF16); o2 += 2048
    assert o2 <= ARENA

    def consume_qa(g, j, b):
        act(pf2[:], bank(b).rearrange("p (h c) -> p h c", c=128), AF.Copy, pk(b), [pf2])
        dst = qa_st[:, j, g * 512:(g + 1) * 512].rearrange("p (h c) -> p h c", c=128)
        rope(pf2[:], [pf2.name], 4, cs_own[:, j, :], cs_own.name, t1c, t2c, dst, ("qa_st", j, g))
    proj_stream(0, 1024, range(NT), consume_qa)

    def consume_kva(g, j, b):
        act(pf2[:], bank(b).rearrange("p (h c) -> p h c", c=128), AF.Copy, pk(b), [pf2])
        dst = ka_st[:, j + 1, :].rearrange("p (h c) -> p h c", c=128)
        rope(pf2[:, 0:2, :], [pf2.name], 2, cs_own[:, j, :], cs_own.name, t1c, t2c, dst, ("ka_st", j + 1))
        S.op("dve", lambda e: e.tensor_copy(out=va_st[:, j + 1, :], in_=pf2[:, 2:4, :].rearrange("p h c -> p (h c)")),
             [pf2], [("va_st", j + 1)])
    proj_stream(1024, 512, range(NT), consume_kva)
    S.op("dve", lambda e: e.tensor_copy(out=ka_st[:, 0, :], in_=halo_k[:]), [halo_k], [("ka_st", 0, 0), ("ka_st", 0, 1)])
    S.op("dve", lambda e: e.tensor_copy(out=va_st[:, 0, :], in_=halo_v[:]), [halo_v], [("va_st", 0)])
    pv6 = bankbf(6).rearrange("p (a b) -> p a b", b=128)
    pv67 = bankbf(6, 2).rearrange("p (a b) -> p a b", b=128)
    for s in range(9):
        transposes([ka_st[:, s, g * 128:(g + 1) * 128] for g in range(2)], pv6[:, 0:2, :], identb[:],
                   [("ka_st", s, 0), ("ka_st", s, 1), identb], pk(6))
        act(kaT[:, :, s * 128:(s + 1) * 128], pv6[:, 0:2, :], AF.Copy, pk(6), [("kaT", s)])
    def attn_tile(j):
        qk = [("qa_st", j, g, x) for g in range(2) for x in range(2)]
        transposes([qa_st[:, j, h * 128:(h + 1) * 128] for h in range(8)], pv6[:, 0:8, :], identb[:], qk + [identb], pk(6))
        act(qaT[:], pv6[:, 0:8, :], AF.Copy, pk(6), [qaT])
        mm([ps[:, h * 256:(h + 1) * 256] for h in range(8)],
           [(qaT[:, h, :], kaT[:, h // 4, j * 128:j * 128 + 256]) for h in range(8)],
           [qaT, ("kaT", j), ("kaT", j + 1)], pk(0, 4), flags=[(True, True)] * 8)
        mk = swam[:, 0 if j == 0 else 1, :].unsqueeze(1).broadcast_to([128, 8, 256])
        stt(sm[:], ps[:, 0:2048].rearrange("p (h c) -> p h c", c=256), 128 ** -0.5, mk, ALU.mult, ALU.add,
            pk(0, 4) + [swam], [sm])
        S.op("dve", lambda e: e.tensor_reduce(out=small[:, 0:8], in_=sm[:], axis=AX.X, op=ALU.max), [sm], [("small", 0)])
        tt("dve", small[:, 0:8], small[:, 0:8], sinkb, ALU.max, [("small", 0), tabs], [("small", 0)])
        ts("dve", small[:, 0:8], small[:, 0:8], -1.0, None, ALU.mult, None, [("small", 0)], [("small", 0)])
        for h in range(8):
            act(pe_[:, h, :], sm[:, h, :], AF.Exp, [sm, ("small", 0)], [("pe_", h), ("small", 2, h)],
                bias=small[:, h:h + 1], accum_out=small[:, 16 + h:17 + h])
        tt("dve", small[:, 8:16], sinkb, small[:, 0:8], ALU.add, [("small", 0), tabs], [("small", 1)])
        act(small[:, 8:16], small[:, 8:16], AF.Exp, [("small", 1)], [("small", 1)])
        tt("dve", small[:, 8:16], small[:, 8:16], small[:, 16:24], ALU.add,
           [("small", 1)] + [("small", 2, h) for h in range(8)], [("small", 1)])
        S.op("dve", lambda e: e.reciprocal(out=small[:, 8:16], in_=small[:, 8:16]), [("small", 1)], [("small", 1)])
        transposes([pe_[:, h, kk * 128:(kk + 1) * 128] for h in range(8) for kk in range(2)], pv67, identb[:],
                   [("pe_", h) for h in range(8)] + [identb], pk(6, 2))
        act(pT[:], pv67, AF.Copy, pk(6, 2), [pT])
        outs, pairs, flags = [], [], []
        for h in range(8):
            g = h // 4
            outs += [ps[:, 2048 + h * 128:2048 + (h + 1) * 128]] * 2
            pairs += [(pT[:, 2 * h, :], va_st[:, j, g * 128:(g + 1) * 128]),
                      (pT[:, 2 * h + 1, :], va_st[:, j + 1, g * 128:(g + 1) * 128])]
            flags += [(True, False), (False, True)]
        mm(outs, pairs, [pT, ("va_st", j), ("va_st", j + 1)], pk(4, 2), flags=flags)
        tt("dve", ao[:], ps[:, 2048:3072].rearrange("p (h c) -> p h c", c=128),
           small[:, 8:16].unsqueeze(2).broadcast_to([128, 8, 128]), ALU.mult, pk(4, 2) + [("small", 1)], [ao])
        transposes([ao[:, h, :] for h in range(8)], pv6[:, 0:8, :], identb[:], [ao, identb], pk(6))
        act(aT[:, :, j * 128:(j + 1) * 128], pv6[:, 0:8, :], AF.Copy, pk(6), [("aT", j)])
    sg = at("sg", T2 + 26624, [128, 512], F32)

    def consume_g(g, j, b):
        act(sg[:], bank(b), AF.Silu, pk(b), [sg])
        tt("dve", onm[:, j, g * 512:(g + 1) * 512], onm[:, j, g * 512:(g + 1) * 512], sg[:], ALU.mult,
           [("onm", j), sg], [("onm", j)])

    def g_stream():
        for g in range(4):
            sl = wc[0] % 2
            wc[0] += 1
            S.dma("pool", wsl[sl][:], w_in_t[11 + g], writes=[wsl[sl]])
            for j in range(NT):
                b = nextbank()
                mm(bank(b), [(xT_own[:, kc, j * 128:(j + 1) * 128], wsl[sl][:, kc, :]) for kc in range(16)],
                   [("xTo", j), wsl[sl]], pk(b))
                consume_g(g, j, b)
                yield
    gs_ = g_stream()
    for j in range(NT):
        attn_tile(j)
        for _ in range(4):
            next(gs_, None)
    for _ in gs_:
        pass
    S.barrier()
    for j in range(NT):
        transposes([onm[:, j, c * 128:(c + 1) * 128] for c in range(16)], pv67, identb[:], [("onm", j), identb], pk(6, 2))
        act(rT[:, :, j * 128:(j + 1) * 128], pv67, AF.Copy, pk(6, 2), [("rT", j)])
    S.barrier()

    mT = at("mT", P0 + 32768, [128, 16, TOK], BF16)
    wm = [at(f"wm{i}", TMP + i * 28672, [128, 56, 256], BF16) for i in range(2)]
    o2 = TMP + 57344
    sa = at("sa", o2, [128, 512], F32); o2 += 2048
    sr = at("sr", o2, [128, 512], F32); o2 += 2048
    assert o2 <= ARENA
    for cg in range(8):
        sl = cg % 2
        c0, c1 = cg * 256, (cg + 1) * 256
        S.dma("pool", wm[sl][:, 0:8, :], w_ab_t[cg], writes=[(wm[sl].name, 0)], chan=wm[sl].name + "a")
        S.dma("pool", wm[sl][:, 8:24, :], w_rb_t[cg], writes=[(wm[sl].name, 1)], chan=wm[sl].name + "b")
        S.dma("pool", wm[sl][:, 24:40, :], w_g_t[cg], writes=[(wm[sl].name, 2)], chan=wm[sl].name + "c")
        S.dma("pool", wm[sl][:, 40:56, :], w_g_t[8 + cg], writes=[(wm[sl].name, 3)], chan=wm[sl].name + "d")
        for sub in range(2):
            cc = cg * 2 + sub
            cs_ = slice(sub * 128, (sub + 1) * 128)
            for hf in range(2):
                b0 = 4 * hf
                tk = slice(hf * 512, (hf + 1) * 512)
                tkeys = list(range(hf * 4, hf * 4 + 4))
                mm(bank(b0), [(wm[sl][:, kc, cs_], aT[:, kc, tk]) for kc in range(8)],
                   [(wm[sl].name, 0)] + [("aT", j) for j in tkeys], pk(b0))
                mm(bank(b0 + 1), [(wm[sl][:, 8 + kc, cs_], rT[:, kc, tk]) for kc in range(16)],
                   [(wm[sl].name, 1)] + [("rT", j) for j in tkeys], pk(b0 + 1))
                mm(bank(b0 + 2), [(wm[sl][:, 24 + kc, cs_], xT_own[:, kc, tk]) for kc in range(16)],
                   [(wm[sl].name, 2)] + [("xTo", j) for j in tkeys], pk(b0 + 2))
                mm(bank(b0 + 3), [(wm[sl][:, 40 + kc, cs_], xT_own[:, kc, tk]) for kc in range(16)],
                   [(wm[sl].name, 3)] + [("xTo", j) for j in tkeys], pk(b0 + 3))
                act(sa[:], bank(b0 + 2), AF.Sigmoid, pk(b0 + 2), [sa])
                act(sr[:], bank(b0 + 3), AF.Sigmoid, pk(b0 + 3), [sr])
                tt("dve", sa[:], sa[:], bank(b0), ALU.mult, [sa] + pk(b0), [sa])
                tt("dve", sr[:], sr[:], bank(b0 + 1), ALU.mult, [sr] + pk(b0 + 1), [sr])
                tt("pool", mT[:, cc, tk], sa[:], sr[:], ALU.add, [sa, sr], [("mT", cc, hf)])
    S.barrier()

    hres = at("hres", RT, [128, 8, D], F32)
    wsl2 = [at(f"wsl2{i}", RT + 65536 + i * 16384, [128, 16, 512], BF16) for i in range(2)]
    S.dma("sp", hres[:], xo.rearrange("(j p) d -> p j d", p=128), writes=[("h", j) for j in range(NT)], chan="hres")
    for g in range(4):
        sl = g % 2
        S.dma("pool", wsl2[sl][:], w_out_t[g], writes=[wsl2[sl]])
        for j in range(NT):
            b = nextbank()
            mm(bank(b), [(mT[:, kc, j * 128:(j + 1) * 128], wsl2[sl][:, kc, :]) for kc in range(16)],
               [wsl2[sl]] + [("mT", kc, j // 4) for kc in range(16)], pk(b))
            tt("dve", hres[:, j, g * 512:(g + 1) * 512], hres[:, j, g * 512:(g + 1) * 512], bank(b), ALU.add,
               [("h", j)] + pk(b), [("h", j)])
    S.barrier()
    if stage <= 2:
        S.dma("sp", out.rearrange("(j p) d -> p j d", p=128), hres[:], reads=[("h", j) for j in range(NT)], chan="out")
        S.barrier()
        return nc

    hnT = at("hnT", P0, [128, 16, TOK], BF16)
    R1 = P0 + 32768
    R2 = RT + 65536
    R2SZ = ARENA - R2
    skT = at("skT", 20640, [128, 16, 128], BF16)
    q_all = at("q_all", R1, [128, 8, D], BF16)
    wsl3 = [at(f"wsl3{i}", R2 + i * 16384, [128, 16, 512], BF16) for i in range(2)]
    xnb3 = at("xnb3", R2 + 32768, [128, D], BF16)
    skn = at("skn", R2 + 36864, [128, 16, 128], BF16)
    xnb3b = at("xnb3b", R2 + 40960, [128, D], BF16)
    sq3b = at("sq3b", R2 + 45056, [128, 8], F32)
    S.dma("sp", gvec[:], g_ffn, writes=[gvec])
    S.dma("pool", skn[:], subk.rearrange("p h n d -> n (p h) d"), writes=[skn])
    pv67 = bankbf(6, 2).rearrange("p (a b) -> p a b", b=128)
    transposes([skn[:, i, :] for i in range(16)], pv67, identb[:], [skn, identb], pk(6, 2))
    act(skT[:], pv67, AF.Copy, pk(6, 2), [skT])
    for g in range(4):
        sl = g % 2
        S.dma("pool", wsl3[sl][:], w_pq_t[g], writes=[wsl3[sl]])
        if g == 0:
            for j in range(NT):
                norm_T(hres[:, j, :], ("h", j), [xnb3, xnb3b][j % 2], [sq2, sq3b][j % 2],
                       hnT[:, :, j * 128:(j + 1) * 128], ("hnT", j), tb=6 if j % 2 == 0 else 2)
        for j in range(NT):
            b = nextbank()
            mm(bank(b), [(hnT[:, kc, j * 128:(j + 1) * 128], wsl3[sl][:, kc, :]) for kc in range(16)],
               [("hnT", j), wsl3[sl]], pk(b))
            act(q_all[:, j, g * 512:(g + 1) * 512], bank(b), AF.Copy, pk(b), [("q_all", j)])
    S.barrier()
    S.dma("sp", hd.rearrange("(j p) d -> p j d", p=128), hres[:], reads=[("h", j) for j in range(NT)], writes=["hd"], chan="hd")
    S.dma("sp", qd.rearrange("(j p) d -> p j d", p=128), q_all[:], reads=[("q_all", j) for j in range(NT)], writes=["qd"], chan="qd")
    S.barrier()
    s_sb = at("s_sb", P0 + 122880, [128, 16, 128], F32)
    D0 = [at(f"D0{i}", P0 + 131072 + i * 8192, [128, 16, 128], F32) for i in range(2)]
    URh = [at(f"URh{i}", P0 + 147456 + i * 8192, [128, 16, 256], BF16) for i in range(2)]
    EdT = [at(f"EdT{i}", P0 + 98304 + i * 4096, [128, 16, 128], BF16) for i in range(4)]
    Ag = [at(f"Ag{i}", P0 + 163840 + i * 2048, [128, TOK], BF16) for i in range(4)]
    qsl = [at(f"qsl{i}", P0 + 114688 + i * 4096, [128, D], BF16) for i in range(2)]
    qTj = at("qTj", 768, [128, 16, 128], BF16)
    Eb = [at("Eb0", 8960, [128, 16, 128], BF16), at("Eb1", 13056, [128, 16, 128], BF16)]
    vtop = at("vtop", 28832, [128, 16, 16], F32)
    cand = at("cand", 29856, [128, 16, 16], F32)
    candr = at("candr", 30880, [128, 16, 16], F32)
    best = at("best", 31904, [128, 8, 16], F32)
    rst = at("rst", 32416, [128, 64], F32)
    srep = at("srep", 24736, [128, 128], F32)
    URt = [at(f"URt{i}", P0 + 65536 + i * 8192, [128, 16, 256], BF16) for i in range(4)]
    Gt_one = at("Gt_sb0", P0 + 32768, [128, 128, 128], BF16)
    Gt_bufs = [Gt_one, Gt_one]

    def routing(j):
        qs = qsl[j % 2]
        transposes([qs[:, i * 128:(i + 1) * 128] for i in range(16)], pv67, identb[:], [qs, identb], pk(6, 2))
        act(qTj[:], pv67, AF.Copy, pk(6, 2), [qTj])
        for half in range(2):
            mm([ps[:, bl * 128:(bl + 1) * 128] for bl in range(8)],
               [(qTj[:, half * 8 + bl, :], skT[:, ((half * 8 + bl) % 2) * 8 + (half * 8 + bl) // 2, :]) for bl in range(8)],
               [qTj, skT], pk(0, 2), flags=[(True, True)] * 8)
            act(s_sb[:, half * 8:(half + 1) * 8, :].rearrange("p a b -> p (a b)"), ps[:, 0:1024], AF.Copy, pk(0, 2), [s_sb])
        for blk in range(16):
            S.op("dve", lambda e, blk=blk: e.max(out=vtop[:, blk, 0:8], in_=s_sb[:, blk, :]), [s_sb], [("vtop", blk, 0)])
            S.op("dve", lambda e, blk=blk: e.match_replace(out=srep[:], in_to_replace=vtop[:, blk, 0:8],
                                                           in_values=s_sb[:, blk, :], imm_value=NEG),
                 [s_sb, ("vtop", blk, 0)], [srep])
            S.op("dve", lambda e, blk=blk: e.max(out=vtop[:, blk, 8:16], in_=srep[:]), [srep], [("vtop", blk, 1)])
            if blk % 4 == 3:
                yield
        for h in range(8):
            vk0 = [("vtop", 2 * h, 0), ("vtop", 2 * h, 1)]
            vk1 = [("vtop", 2 * h + 1, 0), ("vtop", 2 * h + 1, 1)]
            tt("dve", cand[:], vtop[:, 2 * h, :].unsqueeze(2).broadcast_to([128, 16, 16]),
               vtop[:, 2 * h + 1, :].unsqueeze(1).broadcast_to([128, 16, 16]), ALU.add, vk0 + vk1, [cand])
            S.op("dve", lambda e, h=h: e.max(out=best[:, h, 0:8], in_=cand[:]), [cand], [("best", h, 0)])
            S.op("dve", lambda e, h=h: e.match_replace(out=candr[:], in_to_replace=best[:, h, 0:8], in_values=cand[:],
                                                       imm_value=NEG), [cand, ("best", h, 0)], [candr])
            S.op("dve", lambda e, h=h: e.max(out=best[:, h, 8:16], in_=candr[:]), [candr], [("best", h, 1)])
            if h % 2 == 1:
                yield
        bk = [("best", h, x) for h in range(8) for x in range(2)]
        ts("dve", rst[:, 0:8], best[:, :, 0], -1.0, None, ALU.mult, None, bk, [("rst", 0)])
        S.op("dve", lambda e: e.tensor_copy(out=rst[:, 24:32], in_=best[:, :, 15]), bk, [("rst", 3)])
        for h in range(8):
            act(candr[:, h, :], best[:, h, :], AF.Exp, bk + [("rst", 0)], [candr, ("rst", 1, h)],
                bias=rst[:, h:h + 1], accum_out=rst[:, 8 + h:9 + h])
        act(rst[:, 32:40], rst[:, 8:16], AF.Ln, [("rst", 1, h) for h in range(8)], [("rst", 4)])
        tt("dve", rst[:, 16:24], rst[:, 0:8], rst[:, 32:40], ALU.subtract, [("rst", 0), ("rst", 4)], [("rst", 2)])
        yield
        def dense_pre(h):
            sl = h % 2
            vk0 = [("vtop", 2 * h, 0), ("vtop", 2 * h, 1)]
            tt("dve", D0[sl][:], s_sb[:, 2 * h + 1, :].unsqueeze(1).broadcast_to([128, 16, 128]),
               vtop[:, 2 * h, :].unsqueeze(2).broadcast_to([128, 16, 128]), ALU.add, [s_sb] + vk0, [D0[sl]])
            act(Eb[sl][:], D0[sl][:], AF.Exp, [D0[sl], ("rst", 2)], [Eb[sl]], bias=rst[:, 16 + h:17 + h])
        dense_pre(0)
        for h in range(8):
            if h + 1 < 8:
                dense_pre(h + 1)
            sl = h % 2
            vk0 = [("vtop", 2 * h, 0), ("vtop", 2 * h, 1)]
            stt(URh[sl][:, :, 128:256], D0[sl][:], rst[:, 24 + h:25 + h], Eb[sl][:], ALU.is_ge, ALU.mult,
                [D0[sl], Eb[sl], ("rst", 3)], [(URh[sl].name, 1)])
            tt("dve", URh[sl][:, :, 0:128], s_sb[:, 2 * h, :].unsqueeze(1).broadcast_to([128, 16, 128]),
               vtop[:, 2 * h, :].unsqueeze(2).broadcast_to([128, 16, 128]), ALU.is_equal, [s_sb] + vk0, [(URh[sl].name, 0)])
            S.dma("pool", URd[j * 128:(j + 1) * 128, h * 16:(h + 1) * 16, :], URh[sl][:],
                  reads=[(URh[sl].name, 0), (URh[sl].name, 1)], writes=[("URd", j, h)], chan=URh[sl].name)
            yield

    gb = [0]

    def bilinear(j):
        Gt_sb = Gt_bufs[j % 2]
        gn = Gt_sb.name
        for sub in range(8):
            sl = sub % 4
            t0 = j * 128 + sub * 16
            S.dma("sp", URt[sl][:], URd[t0:t0 + 16, :, :].rearrange("t k c -> k t c"),
                  reads=[("URd", j, h) for h in range(8)], writes=[URt[sl]])
            for q4 in range(4):
                b = 4 + gb[0] % 2
                gb[0] += 1
                bv = bank(b).rearrange("p (i t) -> p i t", t=4)
                mm([bv[:, :, tq] for tq in range(4)],
                   [(URt[sl][:, q4 * 4 + tq, 128:256], URt[sl][:, q4 * 4 + tq, 0:128]) for tq in range(4)],
                   [URt[sl]], pk(b), flags=[(True, True)] * 4)
                tl = sub * 16 + q4 * 4
                act(Gt_sb[:, :, tl:tl + 4], bv, AF.Copy, pk(b), [(gn, sub, q4)])
                if q4 % 2 == 1:
                    yield
        for qq in range(4):
            S.dma("act", Gd[j, :, qq * 32:(qq + 1) * 32, :], Gt_sb[:, qq * 32:(qq + 1) * 32, :],
                  reads=[(gn, a_, b_) for a_ in range(8) for b_ in range(4)],
                  writes=[("Gd", j, qq)], chan=f"Gd{qq}_{j % 2}")
        yield

    ak = [0]

    def load_Ed(c):
        S.dma("pool", EdT[c % 4][:], Ed_t[c], writes=[EdT[c % 4]])

    def apart():
        load_Ed(0)
        load_Ed(1)
        load_Ed(2)
        for c in range(128):
            s2 = c % 4
            if c + 3 < 128:
                load_Ed(c + 3)
            for hf in range(2):
                b = 2 + ak[0] % 2
                ak[0] += 1
                tk = slice(hf * 512, (hf + 1) * 512)
                mm(bank(b), [(EdT[s2][:, kc, :], hnT[:, kc, tk]) for kc in range(16)],
                   [EdT[s2]] + [("hnT", jt) for jt in range(hf * 4, hf * 4 + 4)], pk(b))
                act(Ag[s2][:, tk], bank(b), AF.Gelu, pk(b), [(Ag[s2].name, hf)])
            S.dma("act", Ad[c, :, :], Ag[s2][:], reads=[(Ag[s2].name, 0), (Ag[s2].name, 1)], writes=[("Ad", c)], chan=Ag[s2].name)
            yield

    def load_q(j):
        S.dma("sp", qsl[j % 2][:], qd[j * 128:(j + 1) * 128, :], reads=["qd"], writes=[qsl[j % 2]])

    ap_ = apart()
    gstep = [0]
    nchunk = [0]
    load_q(0)
    for j in range(NT + 1):
        if j + 1 < NT:
            load_q(j + 1)
        r = routing(j) if j < NT else None
        b_ = bilinear(j - 1) if j >= 1 else None
        step = 0
        while r is not None or b_ is not None:
            if r is not None:
                try:
                    next(r)
                except StopIteration:
                    r = None
            if b_ is not None:
                try:
                    next(b_)
                except StopIteration:
                    b_ = None
            step += 1
            gstep[0] += 1
            while ap_ is not None and nchunk[0] < (gstep[0] * 128) // 150:
                try:
                    next(ap_)
                    nchunk[0] += 1
                except StopIteration:
                    ap_ = None
    while ap_ is not None:
        try:
            next(ap_)
        except StopIteration:
            ap_ = None
    S.barrier()
    S.dma("sp", hres[:], hd.rearrange("(j p) d -> p j d", p=128), reads=["hd"], writes=[("h", j) for j in range(NT)], chan="hres")
    S.dma("sp", gvec[:], g_fin, writes=[gvec])
    Eub = [at(f"Eub{i}", R1 + i * 16384, [128, 4, D], BF16) for i in range(2)]
    o2 = R2
    PT = [at(f"PT{i}", o2 + i * 8192, [128, 4, TOK], BF16) for i in range(2)]; o2 += 16384
    Gtc = [at(f"Gtc{i}", o2 + i * 2048, [128, TOK], BF16) for i in range(4)]; o2 += 8192
    Adc = [at(f"Adc{i}", o2 + i * 2048, [128, TOK], BF16) for i in range(4)]; o2 += 8192
    ot1 = at("ot1", o2, [128, D], F32); o2 += 8192
    assert o2 <= ARENA, o2
    Eu_v = Eu.rearrange("(c e) d -> e c d", e=128)
    yb = [0]
    NCH = 128

    def emit_P(c):
        sg_ = (c // 4) % 2
        cq = c % 4
        tt("dve", PT[sg_][:, cq, :], Adc[c % 4][:], Gtc[c % 4][:], ALU.mult, [Adc[c % 4], Gtc[c % 4]],
           [(PT[sg_].name, cq, 0), (PT[sg_].name, cq, 1)])

    def final_norm(j):
        act(ot1[:], hres[:, j, :], AF.Square, [("h", j)], [ot1, (junk.name, 0)], accum_out=sq2[:, 0:1])
        ts("dve", sq2[:, 1:2], sq2[:, 0:1], 1.0 / D, EPS, ALU.mult, ALU.add, [(junk.name, 0)], [(junk.name, 1)])
        rsqrt(sq2[:, 1:2], (junk.name, 1))
        stt(ot1[:], hres[:, j, :], sq2[:, 1:2], gvec[:], ALU.mult, ALU.mult, [("h", j), (junk.name, 1), gvec], [ot1])
        S.dma("sp", out[j * 128:(j + 1) * 128, :], ot1[:], reads=[ot1], chan="ot1o")

    def emit_B(g, last=False):
        sg_ = g % 2
        for j in range(NT):
            for db in range(4):
                b = yb[0] % 8
                yb[0] += 1
                mm(bank(b), [(PT[sg_][:, cq, j * 128:(j + 1) * 128], Eub[sg_][:, cq, db * 512:(db + 1) * 512]) for cq in range(4)],
                   [Eub[sg_]] + [(PT[sg_].name, cq, j // 4) for cq in range(4)], pk(b))
                tt("dve", hres[:, j, db * 512:(db + 1) * 512], hres[:, j, db * 512:(db + 1) * 512], bank(b), ALU.add,
                   [("h", j)] + pk(b), [("h", j)])
            if last:
                final_norm(j)

    def load_GA(c):
        S.dma("sp", Gtc[c % 4][:].rearrange("p (a b) -> p a b", b=128), Gd.rearrange("jt j i t -> j jt i t")[:, :, c, :],
              reads=[("Gd", jt, c // 32) for jt in range(NT)], writes=[Gtc[c % 4]])
        S.dma("sp", Adc[c % 4][:], Ad[c, :, :], reads=[("Ad", c)], writes=[Adc[c % 4]])

    def load_Eu(g):
        S.dma("pool", Eub[g % 2][:], Eu_v[:, g * 4:(g + 1) * 4, :], writes=[Eub[g % 2]])

    load_Eu(0)
    load_Eu(1)
    for c in range(3):
        load_GA(c)
    for c in range(NCH):
        g = c // 4
        if c + 3 < NCH:
            load_GA(c + 3)
        emit_P(c)
        if c % 4 == 0 and g >= 1:
            emit_B(g - 1)
            if g + 1 < NCH // 4:
                load_Eu(g + 1)
    emit_B(NCH // 4 - 1, last=True)
    S.barrier()
    nc._marks = S.marks
    return nc


def host_tables(p):
    f32 = np.float32
    T0 = p * 1024
    pos = np.clip(np.arange(1152) + T0 - 128, 0, None).astype(f32)
    inv_freq = (1.0 / (f32(10000.0) ** (np.arange(0, 128, 2, dtype=f32) / f32(128)))).astype(f32)
    ang = (pos[:, None] * inv_freq[None, :]).astype(f32)
    cs = np.concatenate([np.cos(ang), np.sin(ang)], axis=1).astype(f32)
    gam = 1.0 - 2.0 ** (-5.0 - np.arange(8, dtype=np.float64))
    lg = np.log(gam)
    tl = np.arange(128, dtype=np.float64)[:, None, None]
    jj = np.arange(8, dtype=np.float64)[None, :, None]
    wdo = (np.exp(lg[None, None, :] * (1023 - (128 * jj + tl))) * 128 ** -0.5).astype(f32)
    coef = np.zeros((128, 4, 8), f32)
    for r in range(4):
        if r < p:
            coef[:, r, :] = np.exp(lg * 1024.0 * (p - 1 - r))[None, :]
    posc = np.arange(128, dtype=np.float64)
    tabs = np.zeros((128, 40), f32)
    tabs[:, 8:16] = np.exp(lg[None, :] * (127 - posc)[:, None]) * 128 ** -0.5
    tabs[:, 16:24] = np.exp(lg[None, :] * (posc + 1)[:, None])
    tabs[:, 24:32] = np.exp(lg * 128)[None, :]
    k = posc[:, None, None]
    t = posc[None, None, :]
    maskT = np.where(t >= k, np.exp(-lg[None, :, None] * (k + 1)), 0.0) * np.ones((128, 8, 128))
    qi = np.arange(128)[:, None]
    kj = np.arange(256)[None, :]
    diff = qi + 128 - kj
    allowed = (diff >= 0) & (diff < 128)
    mg = np.where(allowed, 0.0, NEG).astype(f32)
    m0 = np.where(allowed & (kj >= 128), 0.0, NEG).astype(f32) if p == 0 else mg
    swam = np.stack([m0, mg], axis=1).astype(f32)
    return cs, wdo, coef, tabs, maskT.astype(f32), swam


def make_in_maps(inputs):
    f32 = np.float32
    x = np.asarray(inputs["x"], f32)
    sinks = np.asarray(inputs["attn_sinks"], f32).reshape(8)
    rep = lambda v: np.ascontiguousarray(np.broadcast_to(np.asarray(v, f32).reshape(1, D), (128, D)))
    def tile_w(w, ncol):
        w = np.asarray(w, f32)
        Kd, N = w.shape
        return np.ascontiguousarray(w.reshape(Kd // 128, 128, N // ncol, ncol).transpose(2, 1, 0, 3))

    w_in_full = np.asarray(inputs["w_in"], f32)[0]
    shared = {
        "ident": np.eye(128, dtype=f32),
        "g_attn": rep(inputs["attn_norm"]), "g_ffn": rep(inputs["ffn_norm"]), "g_fin": rep(inputs["final_norm"]),
        "w_in": tile_w(w_in_full, 512),
        "w_g": tile_w(w_in_full[:, 7680:11776], 256),
        "w_ab": tile_w(np.asarray(inputs["w_attn_branch"], f32)[0], 256),
        "w_rb": tile_w(np.asarray(inputs["w_ret_branch"], f32)[0], 256),
        "w_out": tile_w(np.asarray(inputs["w_out"], f32)[0], 512),
        "w_pq": tile_w(np.asarray(inputs["w_peer_query"], f32)[0], 512),
        "subk": np.ascontiguousarray(np.asarray(inputs["peer_sub_keys"], f32)[0]),
        "Ed": np.ascontiguousarray(np.asarray(inputs["peer_expert_down"], f32)[0].reshape(128, 128, 16, 128).transpose(0, 3, 2, 1)),
        "Eu": np.ascontiguousarray(np.asarray(inputs["peer_expert_up"], f32)[0]),
    }
    maps = []
    for c in range(8):
        b, p = c // 4, c % 4
        cs, wdo, coef, tabs, maskT, swam = host_tables(p)
        tabs[:, 0:8] = sinks[None, :]
        xprev = np.zeros((128, D), f32)
        if p:
            xprev[:] = x[b, p * 1024 - 128:p * 1024]
        m = dict(shared)
        m.update({"xo": np.ascontiguousarray(x[b, p * 1024:(p + 1) * 1024]), "xp": xprev, "cs": cs, "wdo": wdo,
                  "coef": coef, "tabs": tabs, "maskT": maskT, "swam": swam})
        maps.append(m)
    return maps


def kernel(**inputs):
    nc = build_nc()
    maps = make_in_maps(inputs)
    res = run_bass_kernel_spmd(nc, maps, core_ids=list(range(8)))
    outs = [np.asarray(r["out"], np.float32) for r in res.results]
    y = np.stack(outs, 0).reshape(2, 4, 1024, D).reshape(2, 4096, D)
    return y
```
